# Optimizing a Trainium2 kernel written in Bass

```python
import math
import jax, jax.numpy as jnp
from jax import lax
import numpy as np

D_MODEL = 1024
BATCH = 2
SEQ = 8192
DEPTH = 1

HYENA_WIDTH = D_MODEL // 2
HYENA_ORDER = 2
SHORT_CONV = 3
FILTER_EMB = 33
FILTER_BANDS = (FILTER_EMB - 1) // 2
FILTER_HIDDEN = 64
FILTER_SIN_FREQ = 1.0
DECAY_FAST_PCT = 0.3
DECAY_SLOW_PCT = 1.5
DECAY_TARGET = 1e-2
N_DIRECTIONS = 2
FNET_WIDTH = D_MODEL // 2
FNET_GROUPS = 4
FNET_GROUP_DIM = FNET_WIDTH // FNET_GROUPS
N_BRANCHES = 2
IN_COLS = (HYENA_ORDER + 1) * HYENA_WIDTH + FNET_WIDTH + N_BRANCHES * D_MODEL
PEER_HEADS = 8
PEER_N_KEYS = 128
PEER_EXPERTS = PEER_N_KEYS * PEER_N_KEYS
PEER_QUERY_DIM = 256
PEER_HALF = PEER_QUERY_DIM // 2
PEER_TOPK = 16
PEER_BLOCK = 128
RMS_EPS = 1e-6

kernel_name = "hyena_fnet_gated_peer_block"


def rms_norm(x, g):
    xf = x.astype(jnp.float32)
    y = xf * lax.rsqrt(jnp.mean(xf * xf, axis=-1, keepdims=True) + RMS_EPS) * g.astype(jnp.float32)
    return y.astype(x.dtype)


def short_conv(u, w, b):
    c = u.shape[-1]
    y = lax.conv_general_dilated(u, w[:, None, :].astype(u.dtype), window_strides=(1,),
                                 padding=((SHORT_CONV // 2, SHORT_CONV // 2),),
                                 dimension_numbers=('NWC', 'WIO', 'NWC'), feature_group_count=c)
    return y + b


def hyena_filter_spectrum(L, w1, b1, w2, b2, w3):
    f32 = jnp.float32
    t = jnp.linspace(0.0, 1.0, L, dtype=f32)[:, None]
    w = (2.0 * math.pi / L) * jnp.arange(L, dtype=f32)[:, None]
    bands = jnp.linspace(1e-4, FILTER_BANDS - 1, FILTER_BANDS, dtype=f32)[None, :]
    z = jnp.concatenate([t, jnp.cos(bands * w), -jnp.sin(bands * w)], axis=-1)
    hdn = jnp.sin(FILTER_SIN_FREQ * (z @ w1.astype(f32) + b1.astype(f32)))
    hdn = jnp.sin(FILTER_SIN_FREQ * (hdn @ w2.astype(f32) + b2.astype(f32)))
    h = (hdn @ w3.astype(f32)).reshape(L, HYENA_ORDER, N_DIRECTIONS, HYENA_WIDTH)
    min_decay = math.log(DECAY_TARGET) / DECAY_SLOW_PCT
    max_decay = math.log(DECAY_TARGET) / DECAY_FAST_PCT
    deltas = jnp.linspace(min_decay, max_decay, HYENA_WIDTH, dtype=f32)
    h = h * jnp.exp(-t[:, :, None, None] * jnp.abs(deltas))
    k_circ = jnp.concatenate([h[:, :, 0],
                              jnp.zeros((1, HYENA_ORDER, HYENA_WIDTH), f32),
                              h[:0:-1, :, 1]], axis=0)
    k_circ = k_circ / jnp.sum(jnp.abs(k_circ), axis=0, keepdims=True)
    return jnp.fft.rfft(k_circ, axis=0)


def fft_long_conv(u, k_f, skip):
    L = u.shape[1]
    uf = u.astype(jnp.float32)
    U = jnp.fft.rfft(uf, n=2 * L, axis=1)
    y = jnp.fft.irfft(U * k_f[None], n=2 * L, axis=1)[:, :L]
    return y + uf * skip.astype(jnp.float32)


def hyena_mixer(proj, conv_w, conv_b, w1, b1, w2, b2, w3, skip):
    L = proj.shape[1]
    u = short_conv(proj, conv_w, conv_b)
    v, x1, x2 = jnp.split(u, HYENA_ORDER + 1, axis=-1)
    k_f = hyena_filter_spectrum(L, w1, b1, w2, b2, w3)
    z = v
    for o, gate in enumerate((x1, x2)):
        z = (gate * fft_long_conv(z, k_f[:, o], skip[o])).astype(proj.dtype)
    return z


def fnet_mixer(f):
    b, L, _ = f.shape
    fg = f.astype(jnp.float32).reshape(b, L, FNET_GROUPS, FNET_GROUP_DIM)
    y = jnp.fft.fft2(fg, axes=(1, 3), norm='ortho').real
    return y.reshape(b, L, FNET_WIDTH).astype(f.dtype)


def peer_block(hb, w_q, sub_keys, u_tab, v_tab):
    T = hb.shape[0]
    q = (hb @ w_q).reshape(T, PEER_HEADS, 2, PEER_HALF)
    s = jnp.einsum('thcd,hckd->thck', q, sub_keys)
    s_top, i_top = lax.top_k(s, PEER_TOPK)
    cand = s_top[:, :, 0, :, None] + s_top[:, :, 1, None, :]
    cand_idx = i_top[:, :, 0, :, None] * PEER_N_KEYS + i_top[:, :, 1, None, :]
    best, pos = lax.top_k(cand.reshape(T, PEER_HEADS, PEER_TOPK * PEER_TOPK), PEER_TOPK)
    idx = jnp.take_along_axis(cand_idx.reshape(T, PEER_HEADS, PEER_TOPK * PEER_TOPK), pos, axis=-1)
    g = jax.nn.softmax(best.astype(jnp.float32), axis=-1)
    u_sel = jnp.take(u_tab, idx, axis=0)
    a = jax.nn.gelu(jnp.einsum('thkd,td->thk', u_sel, hb).astype(jnp.float32), approximate=False)
    v_sel = jnp.take(v_tab, idx, axis=0)
    return jnp.einsum('thk,thkd->td', (g * a).astype(hb.dtype), v_sel)


def peer_ffn(h, w_q, sub_keys, u_tab, v_tab):
    b, L, d = h.shape
    blocks = h.reshape(b * L // PEER_BLOCK, PEER_BLOCK, d)
    out = lax.map(lambda hb: peer_block(hb, w_q, sub_keys, u_tab, v_tab), blocks)
    return out.reshape(b, L, d)


def setup_inputs(seed: int = 0) -> dict:
    key = jax.random.key(seed)
    ks = jax.random.split(key, 24)
    f32 = jnp.float32
    n = lambda k, shape, scale: jax.random.normal(k, shape, f32) * scale
    HW3 = (HYENA_ORDER + 1) * HYENA_WIDTH
    return {
        "x": jax.random.normal(ks[0], (BATCH, SEQ, D_MODEL), f32),
        "norm_mix_g": 1.0 + n(ks[1], (DEPTH, D_MODEL), 0.02),
        "w_in": n(ks[2], (DEPTH, D_MODEL, IN_COLS), D_MODEL ** -0.5),
        "b_in": n(ks[3], (DEPTH, IN_COLS), 0.02),
        "conv_w": n(ks[4], (DEPTH, SHORT_CONV, HW3), SHORT_CONV ** -0.5),
        "conv_b": n(ks[5], (DEPTH, HW3), 0.02),
        "filt_w1": n(ks[6], (DEPTH, FILTER_EMB, FILTER_HIDDEN), FILTER_EMB ** -0.5),
        "filt_b1": n(ks[7], (DEPTH, FILTER_HIDDEN), 0.1),
        "filt_w2": n(ks[8], (DEPTH, FILTER_HIDDEN, FILTER_HIDDEN), FILTER_HIDDEN ** -0.5),
        "filt_b2": n(ks[9], (DEPTH, FILTER_HIDDEN), 0.1),
        "filt_w3": n(ks[10], (DEPTH, FILTER_HIDDEN, HYENA_ORDER * N_DIRECTIONS * HYENA_WIDTH), FILTER_HIDDEN ** -0.5),
        "hyena_skip": n(ks[11], (DEPTH, HYENA_ORDER, HYENA_WIDTH), 1.0),
        "w_hyena_out": n(ks[12], (DEPTH, HYENA_WIDTH, D_MODEL), HYENA_WIDTH ** -0.5),
        "w_fnet_out": n(ks[13], (DEPTH, FNET_WIDTH, D_MODEL), FNET_WIDTH ** -0.5),
        "w_out": n(ks[14], (DEPTH, D_MODEL, D_MODEL), D_MODEL ** -0.5),
        "norm_ffn_g": 1.0 + n(ks[15], (DEPTH, D_MODEL), 0.02),
        "peer_w_q": n(ks[16], (DEPTH, D_MODEL, PEER_HEADS * PEER_QUERY_DIM), D_MODEL ** -0.5),
        "peer_sub_keys": n(ks[17], (DEPTH, PEER_HEADS, 2, PEER_N_KEYS, PEER_HALF), PEER_HALF ** -0.5),
        "peer_u": n(ks[18], (DEPTH, PEER_EXPERTS, D_MODEL), D_MODEL ** -0.5),
        "peer_v": n(ks[19], (DEPTH, PEER_EXPERTS, D_MODEL), PEER_HEADS ** -0.5),
        "norm_final_g": 1.0 + n(ks[20], (D_MODEL,), 0.02),
    }


def reference(x, norm_mix_g, w_in, b_in, conv_w, conv_b, filt_w1, filt_b1, filt_w2, filt_b2, filt_w3,
              hyena_skip, w_hyena_out, w_fnet_out, w_out, norm_ffn_g, peer_w_q, peer_sub_keys,
              peer_u, peer_v, norm_final_g):
    hw3 = (HYENA_ORDER + 1) * HYENA_WIDTH
    for l in range(DEPTH):
        h = rms_norm(x, norm_mix_g[l])
        p = h @ w_in[l] + b_in[l]
        p_hy = p[..., :hw3]
        p_fn = p[..., hw3:hw3 + FNET_WIDTH]
        g_hy, g_fn = jnp.split(p[..., hw3 + FNET_WIDTH:], N_BRANCHES, axis=-1)
        z_hy = hyena_mixer(p_hy, conv_w[l], conv_b[l], filt_w1[l], filt_b1[l], filt_w2[l],
                           filt_b2[l], filt_w3[l], hyena_skip[l])
        z_fn = fnet_mixer(p_fn)
        y_hy = z_hy @ w_hyena_out[l]
        y_fn = z_fn @ w_fnet_out[l]
        merged = jax.nn.sigmoid(g_hy) * y_hy + jax.nn.sigmoid(g_fn) * y_fn
        x = x + merged @ w_out[l]
        h = rms_norm(x, norm_ffn_g[l])
        x = x + peer_ffn(h, peer_w_q[l], peer_sub_keys[l], peer_u[l], peer_v[l])
    return rms_norm(x, norm_final_g)
```

```python
import contextlib
import math
import numpy as np
import ml_dtypes
import concourse.bass as bass
import concourse.mybir as mybir
from concourse.bass_utils import run_bass_kernel_spmd

F32 = mybir.dt.float32
BF16 = mybir.dt.bfloat16
U32 = mybir.dt.uint32
ALU = mybir.AluOpType
ACT = mybir.ActivationFunctionType
AX = mybir.AxisListType

ENGS = ("pe", "act", "dve", "pool", "sp")
NDMA = 48

L = 8192
NFFT = 16384
D = 1024
NCORES = 8

CFG = dict(NG=4, NPAIR=64, NTT=8, NCH=128, DEBUG=False, DO_MIX=True, DO_C=True)


class Prog:
    def __init__(self, nc):
        self.nc = nc
        self.ops = {e: [] for e in ENGS}
        self.cnt = {e: 0 for e in ENGS}
        self.seen = {e: {} for e in ENGS}
        self.lastw = {}
        self.readers = {}
        self.dma_k = 0
        self.dma_tok = [None] * NDMA
        self.sems = None
        self.ninstr = 0

    def begin(self, stack):
        nc = self.nc
        self.sems = {}
        for e in ENGS:
            self.sems[e] = stack.enter_context(nc.semaphore("s_" + e))
        for i in range(NDMA):
            self.sems[("dma", i)] = stack.enter_context(nc.semaphore("s_dma%d" % i))

    def _deps(self, reads, writes):
        deps = []
        for r in reads:
            t = self.lastw.get(r)
            if t is not None:
                deps.append(t)
        for w in writes:
            t = self.lastw.get(w)
            if t is not None:
                deps.append(t)
            deps.extend(self.readers.get(w, ()))
        return deps

    def _wait(self, eng, deps):
        need = {}
        for (k, v) in deps:
            if k == eng and eng == "pe":
                continue
            if need.get(k, 0) < v:
                need[k] = v
        for k, v in need.items():
            if self.seen[eng].get(k, 0) < v:
                self.seen[eng][k] = v
                self.ops[eng].append(("wait", k, v))

    def _commit(self, tok, reads, writes):
        for r in reads:
            self.readers.setdefault(r, []).append(tok)
        for w in writes:
            self.lastw[w] = tok
            self.readers[w] = []

    def op(self, eng, fn, reads=(), writes=()):
        self._wait(eng, self._deps(reads, writes))
        self.cnt[eng] += 1
        tok = (eng, self.cnt[eng])
        self.ops[eng].append(("ins", fn, eng, 1))
        self._commit(tok, reads, writes)
        self.ninstr += 1
        return tok

    def dma(self, eng, out, in_, reads=(), writes=()):
        slot = self.dma_k % NDMA
        use = self.dma_k // NDMA + 1
        self.dma_k += 1
        deps = self._deps(reads, writes)
        if self.dma_tok[slot] is not None:
            deps.append(self.dma_tok[slot])
        self._wait(eng, deps)
        tok = (("dma", slot), 16 * use)
        self.dma_tok[slot] = tok
        self.ops[eng].append(("ins", lambda e: e.dma_start(out=out, in_=in_), ("dma", slot), 16))
        self._commit(tok, reads, writes)
        self.ninstr += 1
        return tok

    def wait_all_dma(self, eng):
        self._wait(eng, [t for t in self.dma_tok if t is not None])

    def barrier(self):
        toks = [(e, self.cnt[e]) for e in ENGS if self.cnt[e] > 0] + [t for t in self.dma_tok if t is not None]
        for e in ENGS:
            self._wait(e, toks)

    def flush(self):
        nc = self.nc
        sems = self.sems
        self.barrier()
        with nc.Block() as block:
            def run(eng_name):
                recs = self.ops[eng_name]

                def body(e):
                    for rec in recs:
                        if rec[0] == "wait":
                            e.wait_ge(sems[rec[1]], rec[2])
                        else:
                            rec[1](e).then_inc(sems[rec[2]], rec[3])
                return body

            block.tensor(run("pe"))
            block.scalar(run("act"))
            block.vector(run("dve"))
            block.gpsimd(run("pool"))
            block.sync(run("sp"))
        self.ops = {e: [] for e in ENGS}


def fsz(t):
    n = 1
    for s in list(t.shape)[1:]:
        n *= int(s)
    return n


def AP(t, off, dims, p0=0, npart=128):
    ps = fsz(t)
    return bass.AP(t, p0 * ps + off, [[ps, npart]] + [[int(s), int(c)] for s, c in dims])


def DAP(t, off, dims):
    return bass.AP(t, int(off), [[int(s), int(c)] for s, c in dims])


def _bf(a):
    return np.ascontiguousarray(np.asarray(a, dtype=np.float32).astype(ml_dtypes.bfloat16))


def _f(a):
    return np.ascontiguousarray(np.asarray(a, dtype=np.float32))


def host_consts(q):
    c = {}
    c["identb"] = _bf(np.eye(128))
    c["identf"] = _f(np.eye(128))
    c["onesf"] = _f(np.ones((128, 128)))
    n = np.arange(128)
    k128 = np.arange(128)
    ang = 2 * np.pi * np.outer(n, k128) / 128.0
    C128, S128 = np.cos(ang), np.sin(ang)
    c["T128"] = _bf(np.concatenate([C128, -S128], 1))
    c["T128b"] = _bf(np.concatenate([S128, C128], 1))
    c["F128c"] = _f(np.concatenate([C128, -S128], 1))
    n2 = np.arange(128) % 64
    ph = 2 * np.pi * np.outer(n2, k128) / L + (np.pi / 2) * q * (n2[:, None] + k128[None, :])
    c["TWF2c"] = _f(np.concatenate([np.cos(ph), np.cos(ph)], 1))
    c["TWF2s"] = _f(np.concatenate([np.sin(ph), np.sin(ph)], 1))
    a64 = np.arange(64)
    a = 2 * np.pi * np.outer(a64, a64) / 64.0
    BDc = np.zeros((128, 128)); BDs = np.zeros((128, 128))
    for h in range(2):
        BDc[h * 64:(h + 1) * 64, h * 64:(h + 1) * 64] = np.cos(a)
        BDs[h * 64:(h + 1) * 64, h * 64:(h + 1) * 64] = np.sin(a)
    c["BDc"] = _bf(BDc); c["BDs"] = _bf(BDs); c["BDsn"] = _bf(-BDs)
    c["RBD1"] = _bf(np.concatenate([BDc, BDs], 1))
    c["RBD2"] = _bf(np.concatenate([-BDs, BDc], 1))
    k256 = np.arange(256)
    n1full = np.arange(256)
    a256 = 2 * np.pi * np.outer(n1full, k256) / 256.0
    T256full = np.concatenate([np.cos(a256), -np.sin(a256)], 1)
    c["T256lo"] = _bf(T256full[:128]); c["T256hi"] = _bf(T256full[128:])
    p = np.arange(128)
    n1map = np.where(p < 128 - 32 * q, p, p + 128)
    c["T256c"] = _bf(T256full[n1map])
    ph = 2 * np.pi * np.outer(n2, k256) / NFFT
    c["TWH2c"] = _f(np.concatenate([np.cos(ph), np.cos(ph)], 1))
    c["TWH2s"] = _f(np.concatenate([np.sin(ph), np.sin(ph)], 1))
    TWTc = np.zeros((128, 2, 256)); TWTs = np.zeros((128, 2, 256))
    for h in range(2):
        k1 = h * 128 + np.arange(128)
        ph = 2 * np.pi * np.outer(k1, n2) / NFFT
        TWTc[:, h, :] = np.concatenate([np.cos(ph), np.cos(ph)], 1)
        TWTs[:, h, :] = np.concatenate([np.sin(ph), np.sin(ph)], 1)
    c["TWT2c"] = _f(TWTc); c["TWT2s"] = _f(TWTs)
    IC = np.zeros((128, 2, 128)); ISn = np.zeros((128, 2, 128))
    for h in range(2):
        k1 = h * 128 + np.arange(128)
        ph = 2 * np.pi * np.outer(k1, n1map) / 256.0
        IC[:, h, :] = np.cos(ph) / NFFT
        ISn[:, h, :] = -np.sin(ph) / NFFT
    c["ICc"] = _bf(IC); c["ISnc"] = _bf(ISn)
    pstar = 127 - 32 * q
    pprev = (128 - 32 * q) % 128
    mnext = np.ones(128); mnext[pstar] = 0
    mprev = np.ones(128); mprev[pprev] = 0
    c["mnext"] = _bf(np.tile(mnext[None, :], (128, 1)))
    c["mprev"] = _bf(np.tile(mprev[None, :], (128, 1)))
    en = np.zeros((1, 128)); en[0, pstar] = 1
    ep = np.zeros((1, 128)); ep[0, pprev] = 1
    c["enext"] = _bf(en); c["eprev"] = _bf(ep)
    t_lin = np.linspace(0.0, 1.0, L)
    wv = (2.0 * math.pi / L) * np.arange(L)
    bands = np.linspace(1e-4, 15.0, 16)
    z = np.concatenate([t_lin[:, None], np.cos(bands[None, :] * wv[:, None]), -np.sin(bands[None, :] * wv[:, None])], 1)
    nn2, nn1 = np.meshgrid(np.arange(64), np.arange(256), indexing="ij")
    npos = (64 * nn1 + nn2).reshape(-1)
    lag = np.where(npos < L, npos, NFFT - npos)
    lag = np.where(npos == L, 0, lag)
    c["ZcT"] = _f(z[lag].T)
    pp, cc2, aa = np.meshgrid(np.arange(128), np.arange(2), np.arange(64), indexing="ij")
    npos = 64 * (pp + 128 * cc2) + aa
    lag = np.where(npos < L, npos, NFFT - npos).astype(np.float64)
    tc = lag / (L - 1)
    tc = np.where(npos == L, 1.0e4, tc)
    c["tcirc"] = _f(tc)
    c["iota128"] = _f(np.tile(np.arange(128)[None, :], (128, 1)))
    c["iota16"] = _f(np.tile(np.arange(16)[None, :], (128, 1)))
    return c


MIN_DECAY = math.log(1e-2) / 1.5
MAX_DECAY = math.log(1e-2) / 0.3
DELTAS = np.abs(np.linspace(MIN_DECAY, MAX_DECAY, 512).astype(np.float32)).astype(np.float64)

CONST_SPECS = [
    ("identb", [128, 128], BF16), ("identf", [128, 128], F32), ("onesf", [128, 128], F32),
    ("T128", [128, 256], BF16), ("T128b", [128, 256], BF16), ("F128c", [128, 256], F32),
    ("TWF2c", [128, 256], F32), ("TWF2s", [128, 256], F32),
    ("BDc", [128, 128], BF16), ("BDs", [128, 128], BF16), ("BDsn", [128, 128], BF16),
    ("RBD1", [128, 256], BF16), ("RBD2", [128, 256], BF16),
    ("T256lo", [128, 512], BF16), ("T256hi", [128, 512], BF16), ("T256c", [128, 512], BF16),
    ("TWH2c", [128, 512], F32), ("TWH2s", [128, 512], F32),
    ("TWT2c", [128, 2, 256], F32), ("TWT2s", [128, 2, 256], F32),
    ("ICc", [128, 2, 128], BF16), ("ISnc", [128, 2, 128], BF16),
    ("mnext", [128, 128], BF16), ("mprev", [128, 128], BF16),
    ("enext", [1, 128], BF16), ("eprev", [1, 128], BF16),
    ("ZcT", [33, 16384], F32), ("tcirc", [128, 2, 64], F32),
    ("iota128", [128, 128], F32), ("iota16", [128, 16], F32),
]

WEIGHT_SPECS = [
    ("x_b", [8192, 1024]), ("gmix", [1, 1024]), ("gffn", [1, 1024]), ("gfin", [1, 1024]),
    ("wa", [4, 128, 8, 384]), ("cw", [4, 3, 384]), ("cb", [4, 1, 384]), ("binh", [4, 1, 384]),
    ("wfT", [4, 128, 1024]), ("bfn", [4, 128, 1]),
    ("fw1", [33, 64]), ("fb1", [64, 1]), ("fw2", [64, 64]), ("fb2", [64, 1]), ("w3", [4, 64, 512]),
    ("skip", [4, 1, 256]),
    ("wg", [16, 128, 1024]), ("bg", [128, 16]), ("wy", [8, 128, 1024]), ("wo", [8, 128, 1024]),
    ("wq", [16, 128, 1024]), ("skT", [128, 2048]), ("ut", [128, 128, 1024]), ("pv", [128, 128, 1024]),
]


def build():
    cfg = CFG
    nc = bass.Bass("TRN2", target_bir_lowering=False)
    din = {}
    for name, shape, dt in CONST_SPECS:
        din[name] = nc.dram_tensor(name, shape, dt, kind="ExternalInput")
    for name, shape in WEIGHT_SPECS:
        din[name] = nc.dram_tensor(name, shape, F32, kind="ExternalInput")
    out_t = nc.dram_tensor("out", [2048, 1024], F32, kind="ExternalOutput")
    dbg = {}
    if cfg["DEBUG"]:
        dbg["u3"] = nc.dram_tensor("dbg_u3", [128, 384, 64], F32, kind="ExternalOutput")
        dbg["zf"] = nc.dram_tensor("dbg_zf", [128, 256, 64], F32, kind="ExternalOutput")
        dbg["zloc"] = nc.dram_tensor("dbg_zloc", [1024, 2048], F32, kind="ExternalOutput")
        dbg["kf"] = nc.dram_tensor("dbg_kf", [128, 2, 2, 512], F32, kind="ExternalOutput")
        dbg["xa"] = nc.dram_tensor("dbg_xa", [2048, 1024], F32, kind="ExternalOutput")
        dbg["mt"] = nc.dram_tensor("dbg_mt", [128, 2048], F32, kind="ExternalOutput")
    zloc = nc.dram_tensor("zloc", [1024, 2048], BF16, kind="Internal")
    WGs = nc.dram_tensor("WGs", [16, 128, 1024], BF16, kind="Internal")
    WYs = nc.dram_tensor("WYs", [8, 128, 1024], BF16, kind="Internal")
    WQs = nc.dram_tensor("WQs", [16, 128, 1024], BF16, kind="Internal")
    UTs = nc.dram_tensor("UTs", [128, 128, 1024], BF16, kind="Internal")
    Vs = nc.dram_tensor("Vs", [128, 128, 1024], BF16, kind="Internal")

    P = Prog(nc)
    with contextlib.ExitStack() as top:
        P.begin(top)

        uid = {"n": 0}

        def SB(stack, name, shape, dt=F32):
            uid["n"] += 1
            return stack.enter_context(nc.sbuf_tensor("sb%d_%s" % (uid["n"], name), shape, dt))

        PB = [top.enter_context(nc.psum_tensor("pb%d" % i, [128, 512], F32)) for i in range(8)]
        PT = PB[7][:].bitcast(BF16)

        identb = SB(top, "identb", [128, 128], BF16)
        identf = SB(top, "identf", [128, 128])
        epsT = SB(top, "epsT", [128, 1])
        onesb = SB(top, "onesb", [1, 128], BF16)
        P.dma("sp", identb[:], din["identb"].ap(), writes=["identb"])
        P.dma("sp", identf[:], din["identf"].ap(), writes=["identf"])
        P.op("dve", lambda e: e.memset(epsT[:], 1e-6), writes=["epsT"])
        P.op("dve", lambda e: e.memset(onesb[:], 1.0), writes=["onesb"])

        def prep_chunks():
            for i in range(16):
                yield din["wg"].ap()[i], WGs.ap()[i], ("WGs", i)
            for i in range(8):
                yield din["wy"].ap()[i], WYs.ap()[i], ("WYs", i)
            for i in range(16):
                yield din["wq"].ap()[i], WQs.ap()[i], ("WQs", i)
            for i in range(cfg["NCH"]):
                yield din["ut"].ap()[i], UTs.ap()[i], ("UTs", i)
                yield din["pv"].ap()[i], Vs.ap()[i], ("Vs", i)

        prep_state = {"it": prep_chunks(), "n": 0, "pend": None, "bufs": None}

        def emit_prep(n):
            p32, p16 = prep_state["bufs"]
            for _ in range(n):
                try:
                    src, dst, key = next(prep_state["it"])
                except StopIteration:
                    src = None
                if src is not None:
                    k = prep_state["n"]; prep_state["n"] += 1
                    P.dma("sp", p32[k % 2][:], src, writes=[("p32", k % 2)])
                if prep_state["pend"] is not None:
                    pdst, pkey, pk_ = prep_state["pend"]
                    P.dma("sp", pdst, p16[pk_ % 2][:], reads=[("p16", pk_ % 2)], writes=[pkey])
                    prep_state["pend"] = None
                if src is None:
                    return
                P.op("act", lambda e, k=k: e.copy(out=p16[k % 2][:], in_=p32[k % 2][:]), reads=[("p32", k % 2)], writes=[("p16", k % 2)])
                prep_state["pend"] = (dst, key, k)

        def rmsnorm_scale(xt, xkey, ss, rs, col, junk, gbc, hb, hkey):
            P.op("act", lambda e: e.activation(out=junk[:], in_=xt, func=ACT.Square, accum_out=ss[:, col:col + 1]),
                 reads=[xkey], writes=["junk", ("ss", col)])
            P.op("act", lambda e: e.activation(out=rs[:, col:col + 1], in_=ss[:, col:col + 1], func=ACT.Sqrt,
                                               scale=1.0 / 1024.0, bias=epsT[:]),
                 reads=[("ss", col), "epsT"], writes=[("rs", col)])
            P.op("dve", lambda e: e.reciprocal(out=rs[:, col:col + 1], in_=rs[:, col:col + 1]),
                 reads=[("rs", col)], writes=[("rs", col)])
            P.op("dve", lambda e: e.scalar_tensor_tensor(out=hb, in0=xt, scalar=rs[:, col:col + 1], in1=gbc[:],
                                                         op0=ALU.mult, op1=ALU.mult),
                 reads=[xkey, ("rs", col), "gbc"], writes=[hkey])

        if cfg["DO_MIX"]:
            with contextlib.ExitStack() as mx:
                cn = {}
                for name, shape, dt in CONST_SPECS:
                    if name in ("identb", "identf", "ZcT", "iota128", "iota16", "enext", "eprev", "mnext", "mprev"):
                        continue
                    cn[name] = SB(mx, "c_" + name, shape, dt)
                    P.dma("sp", cn[name][:], din[name].ap(), writes=["c_" + name])
                mnext = SB(mx, "mnext", [128, 128], BF16)
                mprev = SB(mx, "mprev", [128, 128], BF16)
                enext = SB(mx, "enext", [1, 128], BF16)
                eprev = SB(mx, "eprev", [1, 128], BF16)
                P.dma("sp", mnext[:], din["mnext"].ap(), writes=["mnext"])
                P.dma("sp", mprev[:], din["mprev"].ap(), writes=["mprev"])
                P.dma("sp", enext[:], din["enext"].ap(), writes=["enext"])
                P.dma("sp", eprev[:], din["eprev"].ap(), writes=["eprev"])
                gbc = SB(mx, "gbc_m", [128, 1024])
                P.dma("sp", gbc[:], DAP(din["gmix"], 0, [[0, 128], [1, 1024]]), writes=["gbc"])
                negpi = SB(mx, "negpi", [128, 1])
                P.op("dve", lambda e: e.memset(negpi[:], -math.pi), writes=["negpi"])

                hdn2T = SB(mx, "hdn2T", [64, 16384], BF16)
                with contextlib.ExitStack() as fs:
                    fw1 = SB(fs, "fw1", [33, 64]); fw2 = SB(fs, "fw2", [64, 64])
                    fb1 = SB(fs, "fb1", [64, 1]); fb2 = SB(fs, "fb2", [64, 1])
                    P.dma("sp", fw1[:], din["fw1"].ap(), writes=["fw1"])
                    P.dma("sp", fw2[:], din["fw2"].ap(), writes=["fw2"])
                    P.dma("sp", fb1[:], din["fb1"].ap(), writes=["fb1"])
                    P.dma("sp", fb2[:], din["fb2"].ap(), writes=["fb2"])
                    zc = [SB(fs, "zc%d" % i, [33, 2048]) for i in range(2)]
                    h1 = [SB(fs, "h1_%d" % i, [64, 512]) for i in range(2)]
                    w1t = [SB(fs, "w1t_%d" % i, [64, 512]) for i in range(2)]
                    w2t = [SB(fs, "w2t_%d" % i, [64, 512]) for i in range(2)]

                    def sin_layer(ps, pskey, bias, bkey, tmpa, tmpb, outap, outkeys, k):
                        ta, tb = tmpa[k % 2], tmpb[k % 2]
                        P.op("act", lambda e: e.activation(out=ta[:], in_=ps, func=ACT.Identity, bias=bias[:], scale=1.0),
                             reads=[pskey, bkey], writes=[("sa", k % 2)])
                        P.op("dve", lambda e: e.tensor_scalar(out=tb[:], in0=ta[:], scalar1=math.pi, scalar2=-2 * math.pi,
                                                              op0=ALU.is_gt, op1=ALU.mult),
                             reads=[("sa", k % 2)], writes=[("sb", k % 2)])
                        P.op("pool", lambda e: e.tensor_add(out=tb[:], in0=tb[:], in1=ta[:]),
                             reads=[("sa", k % 2), ("sb", k % 2)], writes=[("sb", k % 2)])
                        P.op("dve", lambda e: e.tensor_scalar(out=ta[:], in0=ta[:], scalar1=-math.pi, scalar2=2 * math.pi,
                                                              op0=ALU.is_lt, op1=ALU.mult),
                             reads=[("sa", k % 2)], writes=[("sa", k % 2)])
                        P.op("pool", lambda e: e.tensor_add(out=tb[:], in0=tb[:], in1=ta[:]),
                             reads=[("sa", k % 2), ("sb", k % 2)], writes=[("sb", k % 2)])
                        P.op("act", lambda e: e.activation(out=outap, in_=tb[:], func=ACT.Sin),
                             reads=[("sb", k % 2)], writes=outkeys)

                    k = 0
                    for ch in range(8):
                        zt = zc[ch % 2]
                        P.dma("sp", zt[:], DAP(din["ZcT"], ch * 2048, [[16384, 33], [1, 2048]]), writes=[("zc", ch % 2)])
                        for s in range(4):
                            psA = AP(PB[k % 2], 0, [[1, 512]], 0, 64)
                            P.op("pe", lambda e, psA=psA, zt=zt, s=s: e.matmul(out=psA, lhsT=fw1[:], rhs=zt[:, s * 512:(s + 1) * 512],
                                                                             start=True, stop=True),
                                 reads=["fw1", ("zc", ch % 2)], writes=[("pb", k % 2)])
                            sin_layer(psA, ("pb", k % 2), fb1, "fb1", w1t, w2t, h1[k % 2][:], [("h1", k % 2)], k)
                            psB = AP(PB[2 + k % 2], 0, [[1, 512]], 0, 64)
                            P.op("pe", lambda e, psB=psB, k=k: e.matmul(out=psB, lhsT=fw2[:], rhs=h1[k % 2][:], start=True, stop=True),
                                 reads=["fw2", ("h1", k % 2)], writes=[("pb", 2 + k % 2)])
                            col = ch * 2048 + s * 512
                            sin_layer(psB, ("pb", 2 + k % 2), fb2, "fb2", w1t, w2t, hdn2T[:, col:col + 512], ["hdn2T"], k)
                            k += 1
                    P.flush()

                U3 = SB(mx, "U3", [128, 384, 64], BF16)

                for gi in range(cfg["NG"]):
                    zf_stack = contextlib.ExitStack()
                    ZF = SB(zf_stack, "ZF", [128, 256, 64], BF16)
                    with contextlib.ExitStack() as pa:
                        WA3 = SB(pa, "WA3", [128, 3, 8, 384], BF16)
                        WF = SB(pa, "WF", [128, 8, 256], BF16)
                        brow_h = SB(pa, "brow_h", [1, 384], BF16)
                        e0row = SB(pa, "e0row", [1, 384], BF16)
                        e2row = SB(pa, "e2row", [1, 384], BF16)
                        brow_f = SB(pa, "brow_f", [1, 256], BF16)
                        pp = contextlib.ExitStack()
                        wa_sb = SB(pp, "wa_sb", [128, 8, 384])
                        cwb = SB(pp, "cwb", [128, 3, 384])
                        wfT = SB(pp, "wfT", [128, 1024])
                        bfn = SB(pp, "bfn", [128, 1])
                        rows = SB(pp, "rows", [1, 5, 384])
                        rtmp = SB(pp, "rtmp", [1, 384])
                        P.dma("sp", wa_sb[:], din["wa"].ap()[gi], writes=["wa_sb"])
                        P.dma("sp", cwb[:], DAP(din["cw"], gi * 1152, [[0, 128], [384, 3], [1, 384]]), writes=["cwb"])
                        P.dma("sp", wfT[:], din["wfT"].ap()[gi], writes=["wfT"])
                        P.dma("sp", bfn[:], din["bfn"].ap()[gi], writes=["bfn"])
                        P.dma("sp", rows[0:1, 0, :], din["cb"].ap()[gi], writes=["rows0"])
                        P.dma("sp", rows[0:1, 1, :], din["binh"].ap()[gi], writes=["rows1"])
                        P.dma("sp", rows[0:1, 2:5, :], DAP(din["cw"], gi * 1152, [[0, 1], [384, 3], [1, 384]]), writes=["rows2"])
                        for j in range(3):
                            eng = ("dve", "pool", "dve")[j]
                            P.op(eng, lambda e, j=j: e.tensor_tensor(out=WA3[:, j, :, :], in0=wa_sb[:],
                                                                      in1=AP(cwb, j * 384, [[0, 8], [1, 384]]), op=ALU.mult),
                                 reads=["wa_sb", "cwb"], writes=[("WA3", j)])
                        for dc in range(8):
                            P.op("pe", lambda e, dc=dc: e.matmul(out=PB[4 + dc % 2][:, 0:256], lhsT=wfT[:, dc * 128:(dc + 1) * 128],
                                                                 rhs=cn["F128c"][:], start=True, stop=True),
                                 reads=["wfT", "c_F128c"], writes=[("pb", 4 + dc % 2)])
                            P.op("act", lambda e, dc=dc: e.copy(out=WF[:, dc, :], in_=PB[4 + dc % 2][:, 0:256]),
                                 reads=[("pb", 4 + dc % 2)], writes=[("WF", dc)])
                        P.op("pe", lambda e: e.matmul(out=PB[6][0:1, 0:256], lhsT=bfn[:, 0:1], rhs=cn["F128c"][:], start=True, stop=True),
                             reads=["bfn", "c_F128c"], writes=[("pb", 6)])
                        P.op("act", lambda e: e.copy(out=brow_f[:], in_=PB[6][0:1, 0:256]), reads=[("pb", 6)], writes=["brow_f"])
                        P.op("dve", lambda e: e.tensor_add(out=rtmp[:], in0=rows[0:1, 2, :], in1=rows[0:1, 3, :]), reads=["rows0", "rows1", "rows2"], writes=["rtmp"])
                        P.op("dve", lambda e: e.tensor_add(out=rtmp[:], in0=rtmp[:], in1=rows[0:1, 4, :]), reads=["rows0", "rows1", "rows2", "rtmp"], writes=["rtmp"])
                        P.op("dve", lambda e: e.tensor_mul(out=rtmp[:], in0=rtmp[:], in1=rows[0:1, 1, :]), reads=["rows0", "rows1", "rows2", "rtmp"], writes=["rtmp"])
                        P.op("dve", lambda e: e.tensor_add(out=brow_h[:], in0=rtmp[:], in1=rows[0:1, 0, :]), reads=["rows0", "rows1", "rows2", "rtmp"], writes=["brow_h"])
                        P.op("dve", lambda e: e.scalar_tensor_tensor(out=e0row[:], in0=rows[0:1, 1, :], scalar=-1.0, in1=rows[0:1, 2, :],
                                                                     op0=ALU.mult, op1=ALU.mult), reads=["rows0", "rows1", "rows2"], writes=["e0row"])
                        P.op("dve", lambda e: e.scalar_tensor_tensor(out=e2row[:], in0=rows[0:1, 1, :], scalar=-1.0, in1=rows[0:1, 4, :],
                                                                     op0=ALU.mult, op1=ALU.mult), reads=["rows0", "rows1", "rows2"], writes=["e2row"])

                        P.flush()
                        pp.close()
                        xt = [SB(pa, "xt%d" % i, [128, 1024]) for i in range(2)]
                        junk = SB(pa, "junk", [128, 1024], BF16)
                        hb = [SB(pa, "hb%d" % i, [128, 1024], BF16) for i in range(2)]
                        ss = SB(pa, "ss", [128, 2]); rs = SB(pa, "rs", [128, 2])
                        hTr = [SB(pa, "hTr%d" % i, [128, 8, 128], BF16) for i in range(4)]
                        hT0 = SB(pa, "hT0", [128, 8, 128], BF16)
                        hT63 = SB(pa, "hT63", [128, 8, 128], BF16)
                        hTm1 = SB(pa, "hTm1", [128, 8, 128], BF16)
                        hT64 = SB(pa, "hT64", [128, 8, 128], BF16)

                        slot = {}

                        def produce(a, k):
                            x_ap = DAP(din["x_b"], a * 1024, [[64 * 1024, 128], [1, 1024]])
                            P.dma("sp", xt[k % 2][:], x_ap, writes=[("xt", k % 2)])
                            rmsnorm_scale(xt[k % 2][:], ("xt", k % 2), ss, rs, k % 2, junk, gbc, hb[k % 2][:], ("hb", k % 2))
                            for dc in range(8):
                                P.op("pe", lambda e, dc=dc, k=k: e.transpose(out=PT[:, dc * 128:(dc + 1) * 128],
                                                                             in_=hb[k % 2][:, dc * 128:(dc + 1) * 128], identity=identb[:]),
                                     reads=[("hb", k % 2), "identb"], writes=["PT"])
                            if a == 0:
                                dst, key = hT0, "hT0"
                            elif a == 63:
                                dst, key = hT63, "hT63"
                            else:
                                dst, key = hTr[a % 4], ("hTr", a % 4)
                            slot[a] = (dst, key)
                            P.op("act", lambda e, dst=dst: e.copy(out=AP(dst, 0, [[1, 1024]]), in_=PT[:]), reads=["PT"], writes=[key])

                        def mm(a, k):
                            ph = PB[k % 2]; pf = PB[2 + k % 2]
                            srcs = [slot[a - 1] if a > 0 else (hTm1, "hTm1"), slot[a], slot[a + 1] if a < 63 else (hT64, "hT64")]
                            first = True
                            for j in range(3):
                                src, skey = srcs[j]
                                for dc in range(8):
                                    P.op("pe", lambda e, src=src, j=j, dc=dc, first=first, ph=ph: e.matmul(
                                        out=ph[:, 0:384], lhsT=src[:, dc, :], rhs=WA3[:, j, dc, :], start=first, stop=False),
                                        reads=[skey, ("WA3", j)], writes=[("pb", k % 2)])
                                    first = False
                            last = a not in (0, 63)
                            P.op("pe", lambda e, ph=ph, last=last: e.matmul(out=ph[:, 0:384], lhsT=onesb[:], rhs=brow_h[:], start=False, stop=last),
                                 reads=["onesb", "brow_h"], writes=[("pb", k % 2)])
                            if a == 0:
                                P.op("pe", lambda e, ph=ph: e.matmul(out=ph[:, 0:384], lhsT=eprev[:], rhs=e0row[:], start=False, stop=True),
                                     reads=["eprev", "e0row"], writes=[("pb", k % 2)])
                            if a == 63:
                                P.op("pe", lambda e, ph=ph: e.matmul(out=ph[:, 0:384], lhsT=enext[:], rhs=e2row[:], start=False, stop=True),
                                     reads=["enext", "e2row"], writes=[("pb", k % 2)])
                            src, skey = slot[a]
                            for dc in range(8):
                                P.op("pe", lambda e, src=src, dc=dc, pf=pf: e.matmul(out=pf[:, 0:256], lhsT=src[:, dc, :], rhs=WF[:, dc, :],
                                                                                    start=(dc == 0), stop=False),
                                     reads=[skey, ("WF", dc)], writes=[("pb", 2 + k % 2)])
                            P.op("pe", lambda e, pf=pf: e.matmul(out=pf[:, 0:256], lhsT=onesb[:], rhs=brow_f[:], start=False, stop=True),
                                 reads=["onesb", "brow_f"], writes=[("pb", 2 + k % 2)])
                            P.op("dve", lambda e, ph=ph, a=a: e.tensor_copy(out=AP(U3, a, [[64, 384]]), in_=ph[:, 0:384]),
                                 reads=[("pb", k % 2)], writes=["U3"])
                            P.op("act", lambda e, pf=pf, a=a: e.copy(out=AP(ZF, a, [[64, 256]]), in_=pf[:, 0:256]),
                                 reads=[("pb", 2 + k % 2)], writes=["ZF"])

                        produce(63, 0)
                        P.op("dve", lambda e: e.tensor_tensor(out=hTm1[:, :, 1:128], in0=hT63[:, :, 0:127],
                                                              in1=AP(mprev, 1, [[0, 8], [1, 127]]), op=ALU.mult),
                             reads=["hT63", "mprev"], writes=["hTm1"])
                        P.op("dve", lambda e: e.tensor_tensor(out=hTm1[:, :, 0:1], in0=hT63[:, :, 127:128],
                                                              in1=AP(mprev, 0, [[0, 8], [1, 1]]), op=ALU.mult),
                             reads=["hT63", "mprev"], writes=["hTm1"])
                        produce(0, 1)
                        P.op("dve", lambda e: e.tensor_tensor(out=hT64[:, :, 0:127], in0=hT0[:, :, 1:128],
                                                              in1=AP(mnext, 0, [[0, 8], [1, 127]]), op=ALU.mult),
                             reads=["hT0", "mnext"], writes=["hT64"])
                        P.op("dve", lambda e: e.tensor_tensor(out=hT64[:, :, 127:128], in0=hT0[:, :, 0:1],
                                                              in1=AP(mnext, 127, [[0, 8], [1, 1]]), op=ALU.mult),
                             reads=["hT0", "mnext"], writes=["hT64"])
                        kk = 2
                        for a in range(1, 63):
                            produce(a, kk); kk += 1
                            mm(a - 1, a - 1)
                        mm(62, 62)
                        mm(63, 63)
                        if cfg["DEBUG"] and gi == 0 and cfg.get("DUMP_A", False):
                            with contextlib.ExitStack() as ds:
                                d32 = SB(ds, "d32", [128, 64 * 64])
                                for cch in range(6):
                                    P.op("dve", lambda e, cch=cch: e.tensor_copy(out=d32[:], in_=AP(U3, cch * 4096, [[1, 4096]])), reads=["U3"], writes=["d32"])
                                    P.dma("sp", DAP(dbg["u3"], cch * 4096, [[384 * 64, 128], [1, 4096]]), d32[:], reads=["d32"])
                                for cch in range(4):
                                    P.op("dve", lambda e, cch=cch: e.tensor_copy(out=d32[:], in_=AP(ZF, cch * 4096, [[1, 4096]])), reads=["ZF"], writes=["d32"])
                                    P.dma("sp", DAP(dbg["zf"], cch * 4096, [[256 * 64, 128], [1, 4096]]), d32[:], reads=["d32"])
                                P.flush()
                        P.flush()

                    with contextlib.ExitStack() as pf_:
                        ZFN = SB(pf_, "ZFN", [128, 64, 128], BF16)
                        ta_ = [SB(pf_, "fta%d" % i, [128, 256]) for i in range(2)]
                        tb_ = [SB(pf_, "ftb%d" % i, [128, 256]) for i in range(2)]
                        Bre = [SB(pf_, "fBre%d" % i, [128, 4, 128], BF16) for i in range(2)]
                        Bim = [SB(pf_, "fBim%d" % i, [128, 4, 128], BF16) for i in range(2)]
                        k = 0
                        for jb in range(16):
                            for jj in range(4):
                                j = 4 * jb + jj
                                pA = PB[k % 2]
                                P.op("pe", lambda e, pA=pA, j=j: e.matmul(out=pA[:, 0:256], lhsT=AP(ZF, 2 * j * 64, [[1, 128]]), rhs=cn["T128"][:],
                                                                           start=True, stop=False),
                                     reads=["ZF", "c_T128"], writes=[("pb", k % 2)])
                                P.op("pe", lambda e, pA=pA, j=j: e.matmul(out=pA[:, 0:256], lhsT=AP(ZF, (128 + 2 * j) * 64, [[1, 128]]), rhs=cn["T128b"][:],
                                                                           start=False, stop=True),
                                     reads=["ZF", "c_T128b"], writes=[("pb", k % 2)])
                                P.op("dve", lambda e, pA=pA, k=k: e.tensor_tensor(out=ta_[k % 2][:], in0=pA[:, 0:256], in1=cn["TWF2c"][:], op=ALU.mult),
                                     reads=[("pb", k % 2), "c_TWF2c"], writes=[("fta", k % 2)])
                                P.op("dve", lambda e, pA=pA, k=k: e.tensor_tensor(out=tb_[k % 2][:], in0=pA[:, 0:256], in1=cn["TWF2s"][:], op=ALU.mult),
                                     reads=[("pb", k % 2), "c_TWF2s"], writes=[("ftb", k % 2)])
                                P.op("pool", lambda e, k=k, jb=jb, jj=jj: e.tensor_add(out=Bre[jb % 2][:, jj, :], in0=ta_[k % 2][:, 0:128], in1=tb_[k % 2][:, 128:256]),
                                     reads=[("fta", k % 2), ("ftb", k % 2)], writes=[("fBre", jb % 2)])
                                P.op("pool", lambda e, k=k, jb=jb, jj=jj: e.tensor_sub(out=Bim[jb % 2][:, jj, :], in0=ta_[k % 2][:, 128:256], in1=tb_[k % 2][:, 0:128]),
                                     reads=[("fta", k % 2), ("ftb", k % 2)], writes=[("fBim", jb % 2)])
                                k += 1
                            pY = PB[2 + jb % 2]
                            P.op("pe", lambda e, pY=pY, jb=jb: e.matmul(out=pY[:], lhsT=cn["BDc"][:], rhs=AP(Bre[jb % 2], 0, [[1, 512]]), start=True, stop=False),
                                 reads=["c_BDc", ("fBre", jb % 2)], writes=[("pb", 2 + jb % 2)])
                            P.op("pe", lambda e, pY=pY, jb=jb: e.matmul(out=pY[:], lhsT=cn["BDs"][:], rhs=AP(Bim[jb % 2], 0, [[1, 512]]), start=False, stop=True),
                                 reads=["c_BDs", ("fBim", jb % 2)], writes=[("pb", 2 + jb % 2)])
                            P.op("act", lambda e, pY=pY, jb=jb: e.activation(out=AP(ZFN, 4 * jb * 128, [[1, 512]]), in_=pY[:], func=ACT.Identity, scale=1.0 / 1024.0),
                                 reads=[("pb", 2 + jb % 2)], writes=["ZFN"])
                        for c2 in range(2):
                            dst = DAP(zloc, (512 + gi * 128 + c2) * 2048, [[128, 16], [2 * 2048, 64], [1, 128]])
                            P.dma("sp", dst, AP(ZFN, 0, [[128, 64], [1, 128]], c2 * 64, 16), reads=["ZFN"], writes=[("zloc", "fn", gi)])
                        P.flush()

                    zf_stack.close()
                    with contextlib.ExitStack() as ph_:
                        ZHY = SB(ph_, "ZHY", [128, 128, 64], BF16)
                        W3g = SB(ph_, "W3g", [64, 512], BF16)
                        skb = SB(ph_, "skb", [128, 256])
                        W3s = SB(ph_, "W3s", [64, 512])
                        P.dma("sp", W3s[:], din["w3"].ap()[gi], writes=["W3s"])
                        P.op("act", lambda e: e.copy(out=W3g[:], in_=W3s[:]), reads=["W3s"], writes=["W3g"])
                        P.dma("sp", skb[:], DAP(din["skip"], gi * 256, [[0, 128], [1, 256]]), writes=["skb"])
                        NL = 3
                        if gi == 0:
                            prep_state["bufs"] = ([SB(ph_, "p32_%d" % i, [128, 1024]) for i in range(2)],
                                                  [SB(ph_, "p16_%d" % i, [128, 1024], BF16) for i in range(2)])
                        DWp = [SB(ph_, "DWp%d" % i, [128, 2, 2, 64]) for i in range(NL)]
                        KC = [SB(ph_, "KC%d" % i, [128, 2, 2, 2, 64], BF16) for i in range(NL)]
                        l1p = [SB(ph_, "l1p%d" % i, [128, 8]) for i in range(NL)]
                        rn = [SB(ph_, "rn%d" % i, [128, 4]) for i in range(NL)]
                        rncol = [SB(ph_, "rncol%d" % i, [128, 2]) for i in range(NL)]
                        ta = [[SB(ph_, "hta%d_%d" % (l, i), [128, 512]) for i in range(2)] for l in range(NL)]
                        tb = [[SB(ph_, "htb%d_%d" % (l, i), [128, 512]) for i in range(2)] for l in range(NL)]
                        Bre = [[SB(ph_, "hBre%d_%d" % (l, i), [128, 256], BF16) for i in range(2)] for l in range(NL)]
                        Bim = [[SB(ph_, "hBim%d_%d" % (l, i), [128, 256], BF16) for i in range(2)] for l in range(NL)]
                        K2 = [SB(ph_, "K2_%d" % i, [128, 2, 2, 512]) for i in range(NL)]
                        Yre = [SB(ph_, "Yre%d" % i, [128, 256], BF16) for i in range(NL)]
                        Yim = [SB(ph_, "Yim%d" % i, [128, 256], BF16) for i in range(NL)]
                        Ere = [SB(ph_, "Ere%d" % i, [128, 2, 128], BF16) for i in range(NL)]
                        Eim = [SB(ph_, "Eim%d" % i, [128, 2, 128], BF16) for i in range(NL)]
                        t1 = [SB(ph_, "gt1_%d" % i, [128, 128]) for i in range(NL)]
                        t2 = [SB(ph_, "gt2_%d" % i, [128, 128]) for i in range(NL)]
                        z1 = [SB(ph_, "z1_%d" % i, [128, 128], BF16) for i in range(NL)]
                        cnt = [0] * NL
                        last_flush = [P.ninstr]
                        PYB = [PB[1], PB[3], PB[5]]

                        def fwd_fft(ln, lhs_list, lkeys, tabs, tkeys):
                            k = cnt[ln]; cnt[ln] += 1
                            kb = k % 2
                            pA = PB[2 * ln]; pakey = ("pbA", ln)
                            pX = PB[2 * ln + 1]; pxkey = ("pbX", ln)
                            n = len(lhs_list)
                            for i in range(n):
                                P.op("pe", lambda e, i=i, n=n: e.matmul(out=pA[:], lhsT=lhs_list[i], rhs=tabs[i][:], start=(i == 0), stop=(i == n - 1)),
                                     reads=list(lkeys) + [tkeys[i]], writes=[pakey])
                            yield
                            P.op("dve", lambda e: e.tensor_tensor(out=ta[ln][kb][:], in0=pA[:], in1=cn["TWH2c"][:], op=ALU.mult),
                                 reads=[pakey, "c_TWH2c"], writes=[("hta", ln, kb)])
                            P.op("dve", lambda e: e.tensor_tensor(out=tb[ln][kb][:], in0=pA[:], in1=cn["TWH2s"][:], op=ALU.mult),
                                 reads=[pakey, "c_TWH2s"], writes=[("htb", ln, kb)])
                            yield
                            P.op("pool", lambda e: e.tensor_add(out=Bre[ln][kb][:], in0=ta[ln][kb][:, 0:256], in1=tb[ln][kb][:, 256:512]),
                                 reads=[("hta", ln, kb), ("htb", ln, kb)], writes=[("hBre", ln, kb)])
                            P.op("pool", lambda e: e.tensor_sub(out=Bim[ln][kb][:], in0=ta[ln][kb][:, 256:512], in1=tb[ln][kb][:, 0:256]),
                                 reads=[("hta", ln, kb), ("htb", ln, kb)], writes=[("hBim", ln, kb)])
                            yield
                            seq = [(0, "BDc", Bre, "hBre", True, False), (0, "BDs", Bim, "hBim", False, True),
                                   (256, "BDc", Bim, "hBim", True, False), (256, "BDsn", Bre, "hBre", False, True)]
                            for (off, tn, src, sk, st, sp_) in seq:
                                P.op("pe", lambda e, off=off, tn=tn, src=src, st=st, sp_=sp_: e.matmul(
                                    out=pX[:, off:off + 256], lhsT=cn[tn][:], rhs=src[ln][kb][:], start=st, stop=sp_),
                                    reads=["c_" + tn, (sk, ln, kb)], writes=[pxkey])
                            yield

                        def pair_gen(j, ln, jj):
                            pA = PB[2 * ln]; pakey = ("pbA", ln)
                            pX = PB[2 * ln + 1]; pxkey = ("pbX", ln)
                            pYk = pxkey; pNk = pxkey
                            for c2 in range(2):
                                cglob = gi * 128 + 2 * j + c2
                                P.op("act", lambda e, c2=c2, cglob=cglob: e.activation(
                                    out=AP(DWp[ln], c2 * 64, [[128, 2], [1, 64]]), in_=cn["tcirc"][:], func=ACT.Exp, scale=-float(DELTAS[cglob])),
                                    reads=["c_tcirc"], writes=[("DWp", ln)])
                            for chn in range(2):
                                P.op("dve", lambda e, chn=chn: e.tensor_tensor(
                                    out=AP(KC[ln], chn * 256, [[128, 2], [64, 2], [1, 64]]),
                                    in0=AP(PB[6 + chn], 2 * jj, [[4, 2], [1, 2], [8, 64]]),
                                    in1=AP(DWp[ln], chn * 128, [[0, 2], [64, 2], [1, 64]]), op=ALU.mult),
                                    reads=[("pb", 6 + chn), ("DWp", ln)], writes=[("KC", ln)])
                            yield "evac"
                            P.op("dve", lambda e: e.tensor_reduce(out=l1p[ln][:], in_=AP(KC[ln], 0, [[64, 8], [1, 64]]), axis=AX.X, op=ALU.add,
                                                                  apply_absolute_value=True),
                                 reads=[("KC", ln)], writes=[("l1p", ln)])
                            P.op("pe", lambda e: e.matmul(out=PYB[ln][:, 256:264], lhsT=cn["onesf"][:], rhs=l1p[ln][:], start=True, stop=True),
                                 reads=["c_onesf", ("l1p", ln)], writes=[pNk])
                            yield
                            P.op("dve", lambda e: e.tensor_copy(out=rn[ln][:], in_=PYB[ln][:, 256:260]), reads=[pNk], writes=[("rn", ln)])
                            P.op("dve", lambda e: e.tensor_add(out=rn[ln][:], in0=rn[ln][:], in1=PYB[ln][:, 260:264]), reads=[pNk, ("rn", ln)], writes=[("rn", ln)])
                            P.op("dve", lambda e: e.reciprocal(out=rn[ln][:], in_=rn[ln][:]), reads=[("rn", ln)], writes=[("rn", ln)])
                            P.op("pool", lambda e: e.tensor_copy(out=rncol[ln][0:64, :], in_=AP(rn[ln], 0, [[2, 2]], 0, 64)), reads=[("rn", ln)], writes=[("rncol", ln)])
                            P.op("pool", lambda e: e.tensor_copy(out=rncol[ln][64:128, :], in_=AP(rn[ln], 1, [[2, 2]], 64, 64)), reads=[("rn", ln)], writes=[("rncol", ln)])
                            yield
                            for o in range(2):
                                yield from fwd_fft(ln, [AP(KC[ln], o * 128, [[1, 128]]), AP(KC[ln], 256 + o * 128, [[1, 128]])], [("KC", ln)],
                                                   [cn["T256lo"], cn["T256hi"]], ["c_T256lo", "c_T256hi"])
                                for ri in range(2):
                                    P.op("act", lambda e, o=o, ri=ri: e.activation(
                                        out=AP(K2[ln], (o * 2 + ri) * 512, [[256, 2], [1, 256]]),
                                        in_=AP(pX, ri * 256, [[0, 2], [1, 256]]), func=ACT.Identity, scale=rncol[ln][:, o:o + 1]),
                                        reads=[pxkey, ("rncol", ln)], writes=[("K2", ln)])
                                yield
                            if cfg["DEBUG"] and gi == 0 and j == 0:
                                P.dma("sp", DAP(dbg["kf"], 0, [[2048, 128], [1, 2048]]), AP(K2[0], 0, [[1, 2048]]), reads=[("K2", 0)])
                            for o in range(2):
                                if o == 0:
                                    lhs = AP(U3, 2 * j * 64, [[1, 128]]); lk = "U3"
                                else:
                                    lhs = z1[ln][:]; lk = ("z1", ln)
                                yield from fwd_fft(ln, [lhs], [lk], [cn["T256c"]], ["c_T256c"])
                                k = cnt[ln]; cnt[ln] += 1
                                kb = k % 2
                                P.op("dve", lambda e, o=o, kb=kb: e.tensor_tensor(out=ta[ln][kb][:], in0=pX[:], in1=AP(K2[ln], (o * 2) * 512, [[1, 512]]), op=ALU.mult),
                                     reads=[pxkey, ("K2", ln)], writes=[("hta", ln, kb)])
                                P.op("dve", lambda e, o=o, kb=kb: e.tensor_tensor(out=tb[ln][kb][:], in0=pX[:], in1=AP(K2[ln], (o * 2 + 1) * 512, [[1, 512]]), op=ALU.mult),
                                     reads=[pxkey, ("K2", ln)], writes=[("htb", ln, kb)])
                                yield
                                P.op("pool", lambda e, kb=kb: e.tensor_sub(out=Yre[ln][:], in0=ta[ln][kb][:, 0:256], in1=tb[ln][kb][:, 256:512]),
                                     reads=[("hta", ln, kb), ("htb", ln, kb)], writes=[("Yre", ln)])
                                P.op("pool", lambda e, kb=kb: e.tensor_add(out=Yim[ln][:], in0=tb[ln][kb][:, 0:256], in1=ta[ln][kb][:, 256:512]),
                                     reads=[("hta", ln, kb), ("htb", ln, kb)], writes=[("Yim", ln)])
                                yield
                                pD = pA
                                for half in range(2):
                                    P.op("pe", lambda e, half=half: e.matmul(out=pD[:, half * 256:(half + 1) * 256], lhsT=Yre[ln][:, half * 128:(half + 1) * 128],
                                                                             rhs=cn["RBD1"][:], start=True, stop=False),
                                         reads=[("Yre", ln), "c_RBD1"], writes=[pakey])
                                    P.op("pe", lambda e, half=half: e.matmul(out=pD[:, half * 256:(half + 1) * 256], lhsT=Yim[ln][:, half * 128:(half + 1) * 128],
                                                                             rhs=cn["RBD2"][:], start=False, stop=True),
                                         reads=[("Yim", ln), "c_RBD2"], writes=[pakey])
                                yield
                                k = cnt[ln]; cnt[ln] += 1
                                kb = k % 2
                                P.op("dve", lambda e, kb=kb: e.tensor_tensor(out=ta[ln][kb][:], in0=pD[:], in1=AP(cn["TWT2c"], 0, [[1, 512]]), op=ALU.mult),
                                     reads=[pakey, "c_TWT2c"], writes=[("hta", ln, kb)])
                                P.op("dve", lambda e, kb=kb: e.tensor_tensor(out=tb[ln][kb][:], in0=pD[:], in1=AP(cn["TWT2s"], 0, [[1, 512]]), op=ALU.mult),
                                     reads=[pakey, "c_TWT2s"], writes=[("htb", ln, kb)])
                                yield
                                P.op("pool", lambda e, kb=kb: e.tensor_sub(out=Ere[ln][:], in0=AP(ta[ln][kb], 0, [[256, 2], [1, 128]]), in1=AP(tb[ln][kb], 128, [[256, 2], [1, 128]])),
                                     reads=[("hta", ln, kb), ("htb", ln, kb)], writes=[("Ere", ln)])
                                P.op("pool", lambda e, kb=kb: e.tensor_add(out=Eim[ln][:], in0=AP(ta[ln][kb], 128, [[256, 2], [1, 128]]), in1=AP(tb[ln][kb], 0, [[256, 2], [1, 128]])),
                                     reads=[("hta", ln, kb), ("htb", ln, kb)], writes=[("Eim", ln)])
                                yield
                                M = 128 if o == 0 else 32
                                pY = PYB[ln]
                                seq = [("ICc", Ere, "Ere", 0), ("ICc", Ere, "Ere", 1), ("ISnc", Eim, "Eim", 0), ("ISnc", Eim, "Eim", 1)]
                                for i, (tn, src, sk, half) in enumerate(seq):
                                    P.op("pe", lambda e, tn=tn, src=src, half=half, i=i, M=M: e.matmul(
                                        out=pY[0:M, 0:128], lhsT=cn[tn][:, half, 0:M], rhs=src[ln][:, half, :], start=(i == 0), stop=(i == 3)),
                                        reads=["c_" + tn, (sk, ln)], writes=[pYk])
                                yield
                                if o == 0:
                                    P.op("pool", lambda e: e.tensor_tensor(out=AP(t1[ln], 0, [[64, 2], [1, 64]]), in0=AP(U3, 2 * j * 64, [[64, 2], [1, 64]]),
                                                                          in1=AP(skb, 2 * j, [[1, 2], [0, 64]]), op=ALU.mult),
                                         reads=["U3", "skb"], writes=[("gt1", ln)])
                                    P.op("dve", lambda e: e.tensor_add(out=t2[ln][:], in0=pY[:, 0:128], in1=t1[ln][:]), reads=[pYk, ("gt1", ln)], writes=[("gt2", ln)])
                                    P.op("pool", lambda e: e.tensor_mul(out=z1[ln][:], in0=t2[ln][:], in1=AP(U3, (128 + 2 * j) * 64, [[1, 128]])),
                                         reads=[("gt2", ln), "U3"], writes=[("z1", ln)])
                                else:
                                    P.op("pool", lambda e: e.tensor_tensor(out=AP(t1[ln], 0, [[64, 2], [1, 64]], 0, 32), in0=AP(z1[ln], 0, [[64, 2], [1, 64]], 0, 32),
                                                                          in1=AP(skb, 128 + 2 * j, [[1, 2], [0, 64]], 0, 32), op=ALU.mult),
                                         reads=[("z1", ln), "skb"], writes=[("gt1", ln)])
                                    P.op("dve", lambda e: e.tensor_add(out=t2[ln][0:32, :], in0=pY[0:32, 0:128], in1=t1[ln][0:32, :]), reads=[pYk, ("gt1", ln)], writes=[("gt2", ln)])
                                    P.op("pool", lambda e: e.tensor_mul(out=AP(ZHY, 2 * j * 64, [[1, 128]], 0, 32), in0=t2[ln][0:32, :],
                                                                      in1=AP(U3, (256 + 2 * j) * 64, [[1, 128]], 0, 32)),
                                         reads=[("gt2", ln), "U3"], writes=["ZHY"])
                                yield

                        def taps(jp):
                            for chn in range(2):
                                for n2 in range(64):
                                    P.op("pe", lambda e, chn=chn, n2=n2, jp=jp: e.matmul(
                                        out=PB[6 + chn][:, n2 * 8:(n2 + 1) * 8],
                                        lhsT=hdn2T[:, n2 * 256 + chn * 128: n2 * 256 + chn * 128 + 128],
                                        rhs=AP(W3g, chn * 128 + 4 * jp, [[256, 2], [1, 4]], 0, 64), start=True, stop=True),
                                        reads=["hdn2T", "W3g"], writes=[("pb", 6 + chn)])

                        npair = cfg["NPAIR"]
                        free_lanes = list(range(NL))
                        active = []
                        nextj = 0
                        while nextj < npair or active:
                            while free_lanes and nextj < npair:
                                j = nextj; nextj += 1
                                if j % 2 == 0:
                                    taps(j // 2)
                                    if gi == 0:
                                        emit_prep(10)
                                ln = free_lanes.pop(0)
                                g_ = pair_gen(j, ln, j % 2)
                                next(g_)
                                active.append((g_, ln))
                            for (g_, ln) in list(active):
                                try:
                                    next(g_)
                                except StopIteration:
                                    active.remove((g_, ln))
                                    free_lanes.append(ln)
                            if P.ninstr - last_flush[0] > 6000:
                                P.flush(); last_flush[0] = P.ninstr
                        if gi == 0:
                            emit_prep(400)
                        for cq in range(4):
                            dst = DAP(zloc, (gi * 128 + cq * 32) * 2048, [[64, 32], [2048, 32], [1, 64]])
                            P.dma("sp", dst, AP(ZHY, cq * 32 * 64, [[64, 32], [1, 64]], 0, 32), reads=["ZHY"], writes=[("zloc", "hy", gi, cq)])
                        P.flush()

        if cfg["DEBUG"]:
            with contextlib.ExitStack() as ds:
                zb = SB(ds, "dzb", [128, 8, 2048], BF16)
                zf32 = SB(ds, "dzf", [128, 8, 2048])
                rk = [("zloc", "fn", gi) for gi in range(cfg["NG"])] + [("zloc", "hy", gi, cq) for gi in range(cfg["NG"]) for cq in range(4)]
                P.dma("sp", zb[:], DAP(zloc, 0, [[2048, 128], [128 * 2048, 8], [1, 2048]]), reads=rk, writes=["dzb"])
                P.op("dve", lambda e: e.tensor_copy(out=zf32[:], in_=zb[:]), reads=["dzb"], writes=["dzf"])
                P.dma("sp", DAP(dbg["zloc"], 0, [[2048, 128], [128 * 2048, 8], [1, 2048]]), zf32[:], reads=["dzf"])
                P.flush()

        if cfg["DO_C"]:
            with contextlib.ExitStack() as cs:
                gbc = SB(cs, "gbc_c", [128, 1024])
                gbf = SB(cs, "gbf_c", [128, 1024])
                gfin = SB(cs, "gfin_c", [128, 1024])
                P.dma("sp", gbc[:], DAP(din["gmix"], 0, [[0, 128], [1, 1024]]), reads=[], writes=["gbc"])
                P.dma("sp", gbf[:], DAP(din["gffn"], 0, [[0, 128], [1, 1024]]), writes=["gbf"])
                P.dma("sp", gfin[:], DAP(din["gfin"], 0, [[0, 128], [1, 1024]]), writes=["gfin"])
                bg = SB(cs, "bg", [128, 16])
                P.dma("sp", bg[:], din["bg"].ap(), writes=["bg"])
                WO = SB(cs, "WO", [128, 8, 1024], BF16)
                SKT = SB(cs, "SKT", [128, 2048], BF16)
                iota128 = SB(cs, "iota128", [128, 128]); iota16 = SB(cs, "iota16", [128, 16])
                P.dma("sp", iota128[:], din["iota128"].ap(), writes=["iota128"])
                P.dma("sp", iota16[:], din["iota16"].ap(), writes=["iota16"])

                xres = SB(cs, "xres", [128, 2, 1024])
                junk = SB(cs, "junkc", [128, 1024], BF16)
                hbm = SB(cs, "hbm", [128, 1024], BF16)
                ss = SB(cs, "ssc", [128, 2]); rs = SB(cs, "rsc", [128, 2])
                hT = SB(cs, "hTc", [128, 8, 256], BF16)
                zT = SB(cs, "zT", [128, 8, 256], BF16)
                wst = [SB(cs, "wst%d" % i, [128, 1024], BF16) for i in range(4)]
                sg = [SB(cs, "sg%d" % i, [128, 256]) for i in range(2)]
                m12 = [SB(cs, "m12_%d" % i, [128, 256]) for i in range(2)]
                mergedT = SB(cs, "mergedT", [128, 8, 256], BF16)
                hbT = SB(cs, "hbT", [128, 8, 256], BF16)
                qT = SB(cs, "qT", [128, 16, 256], BF16)
                sc = SB(cs, "sc", [128, 16, 128])
                wk = SB(cs, "wk", [128, 256])
                stop_ = SB(cs, "stop", [128, 16, 16])
                itopu = SB(cs, "itopu", [128, 16, 16], U32)
                itop = SB(cs, "itop", [128, 16, 16])
                cand = SB(cs, "cand", [128, 8, 256])
                best = SB(cs, "best", [128, 8, 16])
                posu = SB(cs, "posu", [128, 8, 16], U32)
                k1u = SB(cs, "k1u", [128, 128], U32); k2u = SB(cs, "k2u", [128, 128], U32)
                k1f = SB(cs, "k1f", [128, 128]); k2f = SB(cs, "k2f", [128, 128])
                OH = SB(cs, "OH", [128, 128, 16])
                sel = SB(cs, "sel", [128, 3, 128])
                esum = SB(cs, "esum", [128, 8])
                IT = SB(cs, "IT", [128, 3, 256])
                Pm = [SB(cs, "Pm%d" % i, [128, 8, 128], BF16) for i in range(2)]
                Qm = [SB(cs, "Qm%d" % i, [128, 8, 128], BF16) for i in range(2)]
                Gsb = SB(cs, "Gsb", [128, 128, 256], BF16)
                utb = [SB(cs, "utb%d" % i, [128, 8, 128], BF16) for i in range(3)]
                vvb = [SB(cs, "vvb%d" % i, [128, 1024], BF16) for i in range(3)]
                ga = [SB(cs, "ga%d" % i, [128, 256]) for i in range(2)]
                GA = [SB(cs, "GA%d" % i, [128, 256], BF16) for i in range(2)]
                for fc in range(8):
                    P.dma("sp", xres[:, fc % 2, :], din["wo"].ap()[fc], writes=[("xres", fc % 2)])
                    P.op("act", lambda e, fc=fc: e.copy(out=WO[:, fc, :], in_=xres[:, fc % 2, :]), reads=[("xres", fc % 2)], writes=[("WOc", fc)])
                P.dma("sp", AP(cand, 0, [[1, 2048]]), din["skT"].ap(), writes=["cand"])
                P.op("act", lambda e: e.copy(out=SKT[:], in_=AP(cand, 0, [[1, 2048]])), reads=["cand"], writes=["SKT"])
                wctr = {"n": 0}

                def wload(src_t, idx, key):
                    k = wctr["n"]; wctr["n"] += 1
                    P.dma("sp", wst[k % 4][:], src_t.ap()[idx], reads=[key], writes=[("wst", k % 4)])
                    return wst[k % 4], ("wst", k % 4)

                def transposes(src, skey, dstT, dkey, s):
                    for dc in range(8):
                        P.op("pe", lambda e, dc=dc: e.transpose(out=PT[:, dc * 128:(dc + 1) * 128], in_=src[:, dc * 128:(dc + 1) * 128], identity=identb[:]),
                             reads=[skey, "identb"], writes=["PT"])
                    P.op("act", lambda e, s=s: e.copy(out=AP(dstT, s * 128, [[256, 8], [1, 128]]), in_=PT.rearrange("p (a b) -> p a b", a=8)),
                         reads=["PT"], writes=[dkey])

                for tt in range(cfg["NTT"]):
                    T0 = tt * 256
                    for s in range(2):
                        P.dma("sp", xres[:, s, :], DAP(din["x_b"], (T0 + s * 128) * 1024, [[1024, 128], [1, 1024]]), writes=[("xres", s)])
                        rmsnorm_scale(xres[:, s, :], ("xres", s), ss, rs, s, junk, gbc, hbm[:], "hbm")
                        transposes(hbm, "hbm", hT, "hT", s)
                    zkeys = [("zloc", "fn", gi) for gi in range(cfg["NG"])] + [("zloc", "hy", gi, cq) for gi in range(cfg["NG"]) for cq in range(4)]
                    P.dma("sp", zT[:], DAP(zloc, T0, [[2048, 128], [128 * 2048, 8], [1, 256]]), reads=zkeys, writes=["zT"])
                    for fi in range(8):
                        wgh, kgh = wload(WGs, fi, ("WGs", fi))
                        wgf, kgf = wload(WGs, 8 + fi, ("WGs", 8 + fi))
                        wyy, kyy = wload(WYs, fi, ("WYs", fi))
                        pg = PB[0]; py = PB[1]
                        for half, (wt, wk_) in enumerate(((wgh, kgh), (wgf, kgf))):
                            for dc in range(8):
                                P.op("pe", lambda e, half=half, wt=wt, dc=dc: e.matmul(out=pg[:, half * 256:(half + 1) * 256], lhsT=wt[:, dc * 128:(dc + 1) * 128],
                                                                                      rhs=hT[:, dc, :], start=(dc == 0), stop=(dc == 7)),
                                     reads=[wk_, "hT"], writes=[("pb", 0)])
                        for half in range(2):
                            for cc in range(4):
                                P.op("pe", lambda e, half=half, cc=cc, wyy=wyy: e.matmul(out=py[:, half * 256:(half + 1) * 256],
                                                                               lhsT=wyy[:, (half * 4 + cc) * 128:(half * 4 + cc + 1) * 128],
                                                                               rhs=zT[:, half * 4 + cc, :], start=(cc == 0), stop=(cc == 3)),
                                     reads=[kyy, "zT"], writes=[("pb", 1)])
                        for half in range(2):
                            P.op("act", lambda e, half=half, fi=fi: e.activation(out=sg[half][:], in_=pg[:, half * 256:(half + 1) * 256], func=ACT.Sigmoid,
                                                                                bias=bg[:, half * 8 + fi: half * 8 + fi + 1], scale=1.0),
                                 reads=[("pb", 0), "bg"], writes=[("sg", half)])
                            P.op("dve", lambda e, half=half: e.tensor_tensor(out=m12[half][:], in0=sg[half][:], in1=py[:, half * 256:(half + 1) * 256], op=ALU.mult),
                                 reads=[("sg", half), ("pb", 1)], writes=[("m12", half)])
                        P.op("pool", lambda e, fi=fi: e.tensor_add(out=mergedT[:, fi, :], in0=m12[0][:], in1=m12[1][:]),
                             reads=[("m12", 0), ("m12", 1)], writes=["mergedT"])
                    for s in range(2):
                        for half in range(2):
                            po = PB[2 + half]
                            for fc in range(8):
                                P.op("pe", lambda e, s=s, half=half, fc=fc, po=po: e.matmul(out=po[:], lhsT=mergedT[:, fc, s * 128:(s + 1) * 128],
                                                                                          rhs=WO[:, fc, half * 512:(half + 1) * 512], start=(fc == 0), stop=(fc == 7)),
                                     reads=["mergedT", ("WOc", fc)], writes=[("pb", 2 + half)])
                            P.op("dve", lambda e, s=s, half=half, po=po: e.tensor_add(out=xres[:, s, half * 512:(half + 1) * 512],
                                                                                     in0=xres[:, s, half * 512:(half + 1) * 512], in1=po[:]),
                                 reads=[("pb", 2 + half), ("xres", s)], writes=[("xres", s)])
                    if cfg["DEBUG"] and tt == 0:
                        P.op("dve", lambda e: e.tensor_copy(out=AP(cand, 0, [[1, 2048]]), in_=AP(mergedT, 0, [[1, 2048]])), reads=["mergedT"], writes=["cand"])
                        P.dma("sp", dbg["mt"].ap(), AP(cand, 0, [[1, 2048]]), reads=["cand"])
                    if cfg["DEBUG"]:
                        for s in range(2):
                            P.dma("sp", DAP(dbg["xa"], (T0 + s * 128) * 1024, [[1024, 128], [1, 1024]]), xres[:, s, :], reads=[("xres", s)])
                    for s in range(2):
                        rmsnorm_scale(xres[:, s, :], ("xres", s), ss, rs, s, junk, gbf, hbm[:], "hbm")
                        transposes(hbm, "hbm", hbT, "hbT", s)
                    for hc in range(16):
                        wq_, kq_ = wload(WQs, hc, ("WQs", hc))
                        pq = PB[hc % 2]
                        for dc in range(8):
                            P.op("pe", lambda e, wq_=wq_, dc=dc, pq=pq: e.matmul(out=pq[:, 0:256], lhsT=wq_[:, dc * 128:(dc + 1) * 128], rhs=hbT[:, dc, :],
                                                                                start=(dc == 0), stop=(dc == 7)),
                                 reads=[kq_, "hbT"], writes=[("pb", hc % 2)])
                        P.op("act", lambda e, hc=hc, pq=pq: e.copy(out=qT[:, hc, :], in_=pq[:, 0:256]), reads=[("pb", hc % 2)], writes=["qT"])
                    for s in range(2):
                        for hc in range(16):
                            P.op("pe", lambda e, s=s, hc=hc: e.matmul(out=PB[2 + hc // 4][:, (hc % 4) * 128:(hc % 4 + 1) * 128], lhsT=qT[:, hc, s * 128:(s + 1) * 128],
                                                                     rhs=SKT[:, hc * 128:(hc + 1) * 128], start=True, stop=True),
                                 reads=["qT", "SKT"], writes=[("pb", 2 + hc // 4)])
                        for b4 in range(4):
                            P.op("act", lambda e, b4=b4: e.copy(out=AP(sc, b4 * 512, [[1, 512]]), in_=PB[2 + b4][:]), reads=[("pb", 2 + b4)], writes=["sc"])
                        for hc in range(16):
                            P.op("dve", lambda e, hc=hc: e.max(out=stop_[:, hc, 0:8], in_=sc[:, hc, :]), reads=["sc"], writes=["stop"])
                            P.op("dve", lambda e, hc=hc: e.max_index(out=itopu[:, hc, 0:8], in_max=stop_[:, hc, 0:8], in_values=sc[:, hc, :]),
                                 reads=["sc", "stop"], writes=["itopu"])
                            P.op("dve", lambda e, hc=hc: e.match_replace(out=wk[:, 0:128], in_to_replace=stop_[:, hc, 0:8], in_values=sc[:, hc, :], imm_value=-1e30),
                                 reads=["sc", "stop"], writes=["wk"])
                            P.op("dve", lambda e, hc=hc: e.max(out=stop_[:, hc, 8:16], in_=wk[:, 0:128]), reads=["wk"], writes=["stop"])
                            P.op("dve", lambda e, hc=hc: e.max_index(out=itopu[:, hc, 8:16], in_max=stop_[:, hc, 8:16], in_values=wk[:, 0:128]),
                                 reads=["wk", "stop"], writes=["itopu"])
                        P.op("dve", lambda e: e.tensor_copy(out=itop[:], in_=itopu[:]), reads=["itopu"], writes=["itop"])
                        P.op("pool", lambda e: e.tensor_tensor(out=AP(cand, 0, [[256, 8], [16, 16], [1, 16]]),
                                                              in0=AP(stop_, 0, [[32, 8], [1, 16], [0, 16]]),
                                                              in1=AP(stop_, 16, [[32, 8], [0, 16], [1, 16]]), op=ALU.add),
                             reads=["stop"], writes=["cand"])
                        for h in range(8):
                            P.op("dve", lambda e, h=h: e.max(out=best[:, h, 0:8], in_=cand[:, h, :]), reads=["cand"], writes=["best"])
                            P.op("dve", lambda e, h=h: e.max_index(out=posu[:, h, 0:8], in_max=best[:, h, 0:8], in_values=cand[:, h, :]),
                                 reads=["cand", "best"], writes=["posu"])
                            P.op("dve", lambda e, h=h: e.match_replace(out=wk[:], in_to_replace=best[:, h, 0:8], in_values=cand[:, h, :], imm_value=-1e30),
                                 reads=["cand", "best"], writes=["wk"])
                            P.op("dve", lambda e, h=h: e.max(out=best[:, h, 8:16], in_=wk[:]), reads=["wk"], writes=["best"])
                            P.op("dve", lambda e, h=h: e.max_index(out=posu[:, h, 8:16], in_max=best[:, h, 8:16], in_values=wk[:]),
                                 reads=["wk", "best"], writes=["posu"])
                        P.op("dve", lambda e: e.tensor_single_scalar(out=k1u[:], in_=AP(posu, 0, [[1, 128]]), scalar=4, op=ALU.logical_shift_right),
                             reads=["posu"], writes=["k1u"])
                        P.op("dve", lambda e: e.tensor_single_scalar(out=k2u[:], in_=AP(posu, 0, [[1, 128]]), scalar=15, op=ALU.bitwise_and),
                             reads=["posu"], writes=["k2u"])
                        P.op("dve", lambda e: e.tensor_copy(out=k1f[:], in_=k1u[:]), reads=["k1u"], writes=["k1f"])
                        P.op("dve", lambda e: e.tensor_copy(out=k2f[:], in_=k2u[:]), reads=["k2u"], writes=["k2f"])
                        for which, kf_ in enumerate((k1f, k2f)):
                            P.op("dve", lambda e, kf_=kf_: e.tensor_tensor(out=OH[:], in0=AP(kf_, 0, [[1, 128], [0, 16]]),
                                                                            in1=AP(iota16, 0, [[0, 128], [1, 16]]), op=ALU.is_equal),
                                 reads=["k1f", "k2f", "iota16"], writes=["OH"])
                            P.op("pool", lambda e, which=which: e.tensor_tensor(out=AP(OH, 0, [[256, 8], [16, 16], [1, 16]]),
                                                                               in0=AP(OH, 0, [[256, 8], [16, 16], [1, 16]]),
                                                                               in1=AP(itop, which * 16, [[32, 8], [0, 16], [1, 16]]), op=ALU.mult),
                                 reads=["OH", "itop"], writes=["OH"])
                            P.op("dve", lambda e, which=which: e.tensor_reduce(out=sel[:, which, :], in_=OH[:], axis=AX.X, op=ALU.add),
                                 reads=["OH"], writes=[("sel", which)])
                        P.op("dve", lambda e: e.tensor_tensor(out=AP(sel, 256, [[16, 8], [1, 16]]), in0=best[:],
                                                              in1=AP(best, 0, [[16, 8], [0, 16]]), op=ALU.subtract),
                             reads=["best"], writes=[("sel", 2)])
                        P.op("act", lambda e: e.activation(out=sel[:, 2, :], in_=sel[:, 2, :], func=ACT.Exp), reads=[("sel", 2)], writes=[("sel", 2)])
                        P.op("dve", lambda e: e.tensor_reduce(out=esum[:], in_=AP(sel, 256, [[16, 8], [1, 16]]), axis=AX.X, op=ALU.add),
                             reads=[("sel", 2)], writes=["esum"])
                        P.op("dve", lambda e: e.reciprocal(out=esum[:], in_=esum[:]), reads=["esum"], writes=["esum"])
                        P.op("dve", lambda e: e.tensor_tensor(out=AP(sel, 256, [[16, 8], [1, 16]]), in0=AP(sel, 256, [[16, 8], [1, 16]]),
                                                              in1=AP(esum, 0, [[1, 8], [0, 16]]), op=ALU.mult),
                             reads=[("sel", 2), "esum"], writes=[("sel", 2)])
                        for which in range(3):
                            P.op("pe", lambda e, which=which: e.transpose(out=PB[6][:, which * 128:(which + 1) * 128], in_=sel[:, which, :], identity=identf[:]),
                                 reads=[("sel", which), "identf"], writes=[("pb", 6)])
                        P.op("act", lambda e, s=s: e.copy(out=AP(IT, s * 128, [[256, 3], [1, 128]]), in_=AP(PB[6], 0, [[128, 3], [1, 128]])),
                             reads=[("pb", 6)], writes=["IT"])
                    TB = 8

                    def gbuild(hh, blk):
                        t0 = blk * TB
                        bb = blk % 2
                        pG = PB[6 + bb]
                        P.op("dve", lambda e: e.tensor_tensor(out=Pm[bb][:, :, 0:64], in0=AP(iota128, hh * 64, [[0, TB], [1, 64]]),
                                                              in1=AP(IT, t0, [[1, TB], [0, 64]]), op=ALU.is_equal),
                             reads=["iota128", "IT"], writes=[("Pm", bb)])
                        P.op("dve", lambda e: e.tensor_tensor(out=Qm[bb][:], in0=AP(iota128, 0, [[0, TB], [1, 128]]),
                                                              in1=AP(IT, 256 + t0, [[1, TB], [0, 128]]), op=ALU.is_equal),
                             reads=["iota128", "IT"], writes=[("Qm", bb)])
                        P.op("pool", lambda e: e.tensor_tensor(out=Qm[bb][:], in0=Qm[bb][:],
                                                               in1=AP(IT, 512 + t0, [[1, TB], [0, 128]]), op=ALU.mult),
                             reads=[("Qm", bb), "IT"], writes=[("Qm", bb)])
                        for tl in range(TB):
                            P.op("pe", lambda e, tl=tl: e.matmul(out=pG[:, tl * 64:(tl + 1) * 64], lhsT=Qm[bb][:, tl, :], rhs=Pm[bb][:, tl, 0:64],
                                                                 start=True, stop=True),
                                 reads=[("Pm", bb), ("Qm", bb)], writes=[("pb", 6 + bb)])
                        dst = AP(Gsb, hh * 64 * 256 + t0, [[1, TB], [256, 64]])
                        if bb == 0:
                            P.op("act", lambda e: e.copy(out=dst, in_=AP(pG, 0, [[64, TB], [1, 64]])),
                                 reads=[("pb", 6 + bb)], writes=[("Gsb", hh, "a")])
                        else:
                            P.op("dve", lambda e: e.tensor_copy(out=dst, in_=AP(pG, 0, [[64, TB], [1, 64]])),
                                 reads=[("pb", 6 + bb)], writes=[("Gsb", hh, "d")])

                    nblk = 256 // TB
                    for blk in range(nblk):
                        gbuild(0, blk)
                    P.flush()
                    nch = cfg["NCH"]

                    def loadw(i):
                        P.dma("sp", AP(utb[i % 3], 0, [[1, 1024]]), UTs.ap()[i], reads=[("UTs", i)], writes=[("utb", i % 3)])
                        P.dma("sp", vvb[i % 3][:], Vs.ap()[i], reads=[("Vs", i)], writes=[("vvb", i % 3)])

                    def amm(i):
                        pa_ = PB[i % 2]
                        for dc in range(8):
                            P.op("pe", lambda e, i=i, dc=dc, pa_=pa_: e.matmul(out=pa_[:, 0:256], lhsT=utb[i % 3][:, dc, :], rhs=hbT[:, dc, :],
                                                                              start=(dc == 0), stop=(dc == 7)),
                                 reads=[("utb", i % 3), "hbT"], writes=[("pb", i % 2)])

                    loadw(0)
                    if nch > 1:
                        loadw(1)
                    amm(0)
                    for i in range(nch):
                        if i + 2 < nch:
                            loadw(i + 2)
                        if i + 1 < nch:
                            amm(i + 1)
                        P.op("act", lambda e, i=i: e.activation(out=ga[i % 2][:], in_=PB[i % 2][:, 0:256], func=ACT.Gelu),
                             reads=[("pb", i % 2)], writes=[("ga", i % 2)])
                        eng = "dve" if i % 2 == 0 else "pool"
                        hh = i // 64
                        P.op(eng, lambda e, i=i: e.tensor_tensor(out=GA[i % 2][:], in0=ga[i % 2][:], in1=Gsb[:, i, :], op=ALU.mult),
                             reads=[("ga", i % 2), ("Gsb", hh, "a"), ("Gsb", hh, "d")], writes=[("GA", i % 2)])
                        for s in range(2):
                            for half in range(2):
                                P.op("pe", lambda e, i=i, s=s, half=half: e.matmul(out=PB[2 + s * 2 + half][:], lhsT=GA[i % 2][:, s * 128:(s + 1) * 128],
                                                                                  rhs=vvb[i % 3][:, half * 512:(half + 1) * 512],
                                                                                  start=(i == 0), stop=(i == nch - 1)),
                                     reads=[("GA", i % 2), ("vvb", i % 3)], writes=[("pb", 2 + s * 2 + half)])
                        if i < 64 and i % 2 == 0 and (i // 2) < nblk:
                            gbuild(1, i // 2)
                    for s in range(2):
                        for half in range(2):
                            P.op("dve", lambda e, s=s, half=half: e.tensor_add(out=xres[:, s, half * 512:(half + 1) * 512],
                                                                              in0=xres[:, s, half * 512:(half + 1) * 512], in1=PB[2 + s * 2 + half][:]),
                                 reads=[("pb", 2 + s * 2 + half), ("xres", s)], writes=[("xres", s)])
                        P.op("act", lambda e, s=s: e.activation(out=junk[:], in_=xres[:, s, :], func=ACT.Square, accum_out=ss[:, s:s + 1]),
                             reads=[("xres", s)], writes=["junk", ("ss", s)])
                        P.op("act", lambda e, s=s: e.activation(out=rs[:, s:s + 1], in_=ss[:, s:s + 1], func=ACT.Sqrt, scale=1.0 / 1024.0, bias=epsT[:]),
                             reads=[("ss", s), "epsT"], writes=[("rs", s)])
                        P.op("dve", lambda e, s=s: e.reciprocal(out=rs[:, s:s + 1], in_=rs[:, s:s + 1]), reads=[("rs", s)], writes=[("rs", s)])
                        P.op("dve", lambda e, s=s: e.scalar_tensor_tensor(out=AP(sc, s * 1024, [[1, 1024]]), in0=xres[:, s, :], scalar=rs[:, s:s + 1], in1=gfin[:],
                                                                         op0=ALU.mult, op1=ALU.mult),
                             reads=[("xres", s), ("rs", s), "gfin"], writes=["sc"])
                        P.dma("sp", DAP(out_t, (T0 + s * 128) * 1024, [[1024, 128], [1, 1024]]), AP(sc, s * 1024, [[1, 1024]]), reads=["sc"])
                    P.flush()
        P.wait_all_dma("sp")
        P.flush()
    return nc


def make_inputs(x, norm_mix_g, w_in, b_in, conv_w, conv_b, filt_w1, filt_b1, filt_w2, filt_b2, filt_w3,
                hyena_skip, w_hyena_out, w_fnet_out, w_out, norm_ffn_g, peer_w_q, peer_sub_keys,
                peer_u, peer_v, norm_final_g):
    f = lambda a: np.ascontiguousarray(np.asarray(a, dtype=np.float32))
    x = f(x); w_in = f(w_in)[0]; b_in = f(b_in)[0]; conv_w = f(conv_w)[0]; conv_b = f(conv_b)[0]
    shared = {}
    shared["gmix"] = f(norm_mix_g)[0][None, :]
    shared["gffn"] = f(norm_ffn_g)[0][None, :]
    shared["gfin"] = f(norm_final_g)[None, :]
    wa = np.zeros((4, 128, 8, 384), np.float32)
    cw = np.zeros((4, 3, 384), np.float32); cb = np.zeros((4, 1, 384), np.float32); binh = np.zeros((4, 1, 384), np.float32)
    wfT = np.zeros((4, 128, 1024), np.float32); bfn = np.zeros((4, 128, 1), np.float32)
    w3 = np.zeros((4, 64, 512), np.float32); skip = np.zeros((4, 1, 256), np.float32)
    fw3 = f(filt_w3)[0]; hs = f(hyena_skip)[0]
    for g in range(4):
        cols = np.concatenate([np.arange(128) + g * 128 + kind * 512 for kind in range(3)])
        wa[g] = w_in[:, cols].reshape(8, 128, 384).transpose(1, 0, 2)
        cw[g] = conv_w[:, cols]; cb[g, 0] = conv_b[cols]; binh[g, 0] = b_in[cols]
        fc = 1536 + g * 128 + np.arange(128)
        wfT[g] = w_in[:, fc].T; bfn[g, :, 0] = b_in[fc]
        w3c = np.concatenate([o * 1024 + dr * 512 + g * 128 + np.arange(128) for o in range(2) for dr in range(2)])
        w3[g] = fw3[:, w3c]
        skip[g, 0] = hs[:, g * 128:(g + 1) * 128].reshape(-1)
    shared.update(wa=wa, cw=cw, cb=cb, binh=binh, wfT=wfT, bfn=bfn, w3=w3, skip=skip)
    shared["fw1"] = f(filt_w1)[0]; shared["fb1"] = f(filt_b1)[0][:, None]
    shared["fw2"] = f(filt_w2)[0]; shared["fb2"] = f(filt_b2)[0][:, None]
    wgc = w_in[:, 2048:4096]
    shared["wg"] = np.ascontiguousarray(wgc.reshape(8, 128, 16, 128).transpose(2, 1, 0, 3).reshape(16, 128, 1024))
    shared["bg"] = np.ascontiguousarray(b_in[2048:4096].reshape(16, 128).T)
    why = f(w_hyena_out)[0]; wfn = f(w_fnet_out)[0]
    wy = np.zeros((8, 128, 2, 4, 128), np.float32)
    wy[:, :, 0] = why.reshape(4, 128, 8, 128).transpose(2, 1, 0, 3)
    wy[:, :, 1] = wfn.reshape(4, 128, 8, 128).transpose(2, 1, 0, 3)
    shared["wy"] = wy.reshape(8, 128, 1024)
    shared["wo"] = f(w_out)[0].reshape(8, 128, 1024)
    wq = f(peer_w_q)[0]
    shared["wq"] = np.ascontiguousarray(wq.reshape(8, 128, 16, 128).transpose(2, 1, 0, 3).reshape(16, 128, 1024))
    sk = f(peer_sub_keys)[0]
    shared["skT"] = np.ascontiguousarray(sk.reshape(16, 128, 128).transpose(2, 0, 1).reshape(128, 2048))
    pu = f(peer_u)[0]
    shared["ut"] = np.ascontiguousarray(pu.reshape(128, 128, 8, 128).transpose(0, 3, 2, 1).reshape(128, 128, 1024))
    shared["pv"] = f(peer_v)[0].reshape(128, 128, 1024)
    in_maps = []
    for r in range(NCORES):
        b, q = r // 4, r % 4
        m = dict(shared)
        m["x_b"] = np.ascontiguousarray(np.roll(x[b], -2048 * q, axis=0))
        m.update(host_consts(q))
        in_maps.append(m)
    return in_maps


def kernel(**inputs):
    in_maps = make_inputs(**inputs)
    nc = build()
    res = run_bass_kernel_spmd(nc, in_maps, core_ids=list(range(NCORES)))
    out = np.zeros((2, L, D), np.float32)
    for r in range(NCORES):
        b, q = r // 4, r % 4
        out[b, 2048 * q:2048 * (q + 1)] = res.results[r]["out"]
    return out
```

```python
import contextlib
import math
import numpy as np
import ml_dtypes
import concourse.bass as bass
import concourse.mybir as mybir
from concourse.bass_utils import run_bass_kernel_spmd

F32 = mybir.dt.float32
BF16 = mybir.dt.bfloat16
U32 = mybir.dt.uint32
ALU = mybir.AluOpType
ACT = mybir.ActivationFunctionType
AX = mybir.AxisListType

ENGS = ("pe", "act", "dve", "pool", "sp")
NDMA = 48

L = 8192
NFFT = 16384
D = 1024
NCORES = 8

CFG = dict(NG=4, NPAIR=64, NTT=8, NCH=128, DEBUG=False, DO_MIX=True, DO_C=True)


class Prog:
    def __init__(self, nc):
        self.nc = nc
        self.ops = {e: [] for e in ENGS}
        self.cnt = {e: 0 for e in ENGS}
        self.seen = {e: {} for e in ENGS}
        self.lastw = {}
        self.readers = {}
        self.dma_k = 0
        self.dma_tok = [None] * NDMA
        self.sems = None
        self.ninstr = 0

    def begin(self, stack):
        nc = self.nc
        self.sems = {}
        for e in ENGS:
            self.sems[e] = stack.enter_context(nc.semaphore("s_" + e))
        for i in range(NDMA):
            self.sems[("dma", i)] = stack.enter_context(nc.semaphore("s_dma%d" % i))

    def _deps(self, reads, writes):
        deps = []
        for r in reads:
            t = self.lastw.get(r)
            if t is not None:
                deps.append(t)
        for w in writes:
            t = self.lastw.get(w)
            if t is not None:
                deps.append(t)
            deps.extend(self.readers.get(w, ()))
        return deps

    def _wait(self, eng, deps):
        need = {}
        for (k, v) in deps:
            if k == eng and eng == "pe":
                continue
            if need.get(k, 0) < v:
                need[k] = v
        for k, v in need.items():
            if self.seen[eng].get(k, 0) < v:
                self.seen[eng][k] = v
                self.ops[eng].append(("wait", k, v))

    def _commit(self, tok, reads, writes):
        for r in reads:
            self.readers.setdefault(r, []).append(tok)
        for w in writes:
            self.lastw[w] = tok
            self.readers[w] = []

    def op(self, eng, fn, reads=(), writes=()):
        self._wait(eng, self._deps(reads, writes))
        self.cnt[eng] += 1
        tok = (eng, self.cnt[eng])
        self.ops[eng].append(("ins", fn, eng, 1))
        self._commit(tok, reads, writes)
        self.ninstr += 1
        return tok

    def dma(self, eng, out, in_, reads=(), writes=()):
        slot = self.dma_k % NDMA
        use = self.dma_k // NDMA + 1
        self.dma_k += 1
        deps = self._deps(reads, writes)
        if self.dma_tok[slot] is not None:
            deps.append(self.dma_tok[slot])
        self._wait(eng, deps)
        tok = (("dma", slot), 16 * use)
        self.dma_tok[slot] = tok
        self.ops[eng].append(("ins", lambda e: e.dma_start(out=out, in_=in_), ("dma", slot), 16))
        self._commit(tok, reads, writes)
        self.ninstr += 1
        return tok

    def wait_all_dma(self, eng):
        self._wait(eng, [t for t in self.dma_tok if t is not None])

    def barrier(self):
        toks = [(e, self.cnt[e]) for e in ENGS if self.cnt[e] > 0] + [t for t in self.dma_tok if t is not None]
        for e in ENGS:
            self._wait(e, toks)

    def flush(self):
        nc = self.nc
        sems = self.sems
        self.barrier()
        with nc.Block() as block:
            def run(eng_name):
                recs = self.ops[eng_name]

                def body(e):
                    for rec in recs:
                        if rec[0] == "wait":
                            e.wait_ge(sems[rec[1]], rec[2])
                        else:
                            rec[1](e).then_inc(sems[rec[2]], rec[3])
                return body

            block.tensor(run("pe"))
            block.scalar(run("act"))
            block.vector(run("dve"))
            block.gpsimd(run("pool"))
            block.sync(run("sp"))
        self.ops = {e: [] for e in ENGS}


def fsz(t):
    n = 1
    for s in list(t.shape)[1:]:
        n *= int(s)
    return n


def AP(t, off, dims, p0=0, npart=128):
    ps = fsz(t)
    return bass.AP(t, p0 * ps + off, [[ps, npart]] + [[int(s), int(c)] for s, c in dims])


def DAP(t, off, dims):
    return bass.AP(t, int(off), [[int(s), int(c)] for s, c in dims])


def _bf(a):
    return np.ascontiguousarray(np.asarray(a, dtype=np.float32).astype(ml_dtypes.bfloat16))


def _f(a):
    return np.ascontiguousarray(np.asarray(a, dtype=np.float32))


def host_consts(q):
    c = {}
    c["identb"] = _bf(np.eye(128))
    c["identf"] = _f(np.eye(128))
    c["onesf"] = _f(np.ones((128, 128)))
    n = np.arange(128)
    k128 = np.arange(128)
    ang = 2 * np.pi * np.outer(n, k128) / 128.0
    C128, S128 = np.cos(ang), np.sin(ang)
    c["T128"] = _bf(np.concatenate([C128, -S128], 1))
    c["T128b"] = _bf(np.concatenate([S128, C128], 1))
    c["F128c"] = _f(np.concatenate([C128, -S128], 1))
    n2 = np.arange(128) % 64
    ph = 2 * np.pi * np.outer(n2, k128) / L + (np.pi / 2) * q * (n2[:, None] + k128[None, :])
    c["TWF2c"] = _f(np.concatenate([np.cos(ph), np.cos(ph)], 1))
    c["TWF2s"] = _f(np.concatenate([np.sin(ph), np.sin(ph)], 1))
    a64 = np.arange(64)
    a = 2 * np.pi * np.outer(a64, a64) / 64.0
    BDc = np.zeros((128, 128)); BDs = np.zeros((128, 128))
    for h in range(2):
        BDc[h * 64:(h + 1) * 64, h * 64:(h + 1) * 64] = np.cos(a)
        BDs[h * 64:(h + 1) * 64, h * 64:(h + 1) * 64] = np.sin(a)
    c["BDc"] = _bf(BDc); c["BDs"] = _bf(BDs); c["BDsn"] = _bf(-BDs)
    c["RBD1"] = _bf(np.concatenate([BDc, BDs], 1))
    c["RBD2"] = _bf(np.concatenate([-BDs, BDc], 1))
    k256 = np.arange(256)
    n1full = np.arange(256)
    a256 = 2 * np.pi * np.outer(n1full, k256) / 256.0
    T256full = np.concatenate([np.cos(a256), -np.sin(a256)], 1)
    c["T256lo"] = _bf(T256full[:128]); c["T256hi"] = _bf(T256full[128:])
    p = np.arange(128)
    n1map = np.where(p < 128 - 32 * q, p, p + 128)
    c["T256c"] = _bf(T256full[n1map])
    ph = 2 * np.pi * np.outer(n2, k256) / NFFT
    c["TWH2c"] = _f(np.concatenate([np.cos(ph), np.cos(ph)], 1))
    c["TWH2s"] = _f(np.concatenate([np.sin(ph), np.sin(ph)], 1))
    TWTc = np.zeros((128, 2, 256)); TWTs = np.zeros((128, 2, 256))
    for h in range(2):
        k1 = h * 128 + np.arange(128)
        ph = 2 * np.pi * np.outer(k1, n2) / NFFT
        TWTc[:, h, :] = np.concatenate([np.cos(ph), np.cos(ph)], 1)
        TWTs[:, h, :] = np.concatenate([np.sin(ph), np.sin(ph)], 1)
    c["TWT2c"] = _f(TWTc); c["TWT2s"] = _f(TWTs)
    IC = np.zeros((128, 2, 128)); ISn = np.zeros((128, 2, 128))
    for h in range(2):
        k1 = h * 128 + np.arange(128)
        ph = 2 * np.pi * np.outer(k1, n1map) / 256.0
        IC[:, h, :] = np.cos(ph) / NFFT
        ISn[:, h, :] = -np.sin(ph) / NFFT
    c["ICc"] = _bf(IC); c["ISnc"] = _bf(ISn)
    pstar = 127 - 32 * q
    pprev = (128 - 32 * q) % 128
    mnext = np.ones(128); mnext[pstar] = 0
    mprev = np.ones(128); mprev[pprev] = 0
    c["mnext"] = _bf(np.tile(mnext[None, :], (128, 1)))
    c["mprev"] = _bf(np.tile(mprev[None, :], (128, 1)))
    en = np.zeros((1, 128)); en[0, pstar] = 1
    ep = np.zeros((1, 128)); ep[0, pprev] = 1
    c["enext"] = _bf(en); c["eprev"] = _bf(ep)
    t_lin = np.linspace(0.0, 1.0, L)
    wv = (2.0 * math.pi / L) * np.arange(L)
    bands = np.linspace(1e-4, 15.0, 16)
    z = np.concatenate([t_lin[:, None], np.cos(bands[None, :] * wv[:, None]), -np.sin(bands[None, :] * wv[:, None])], 1)
    nn2, nn1 = np.meshgrid(np.arange(64), np.arange(256), indexing="ij")
    npos = (64 * nn1 + nn2).reshape(-1)
    lag = np.where(npos < L, npos, NFFT - npos)
    lag = np.where(npos == L, 0, lag)
    c["ZcT"] = _f(z[lag].T)
    pp, cc2, aa = np.meshgrid(np.arange(128), np.arange(2), np.arange(64), indexing="ij")
    npos = 64 * (pp + 128 * cc2) + aa
    lag = np.where(npos < L, npos, NFFT - npos).astype(np.float64)
    tc = lag / (L - 1)
    tc = np.where(npos == L, 1.0e4, tc)
    c["tcirc"] = _f(tc)
    c["iota128"] = _f(np.tile(np.arange(128)[None, :], (128, 1)))
    c["iota16"] = _f(np.tile(np.arange(16)[None, :], (128, 1)))
    return c


MIN_DECAY = math.log(1e-2) / 1.5
MAX_DECAY = math.log(1e-2) / 0.3
DELTAS = np.abs(np.linspace(MIN_DECAY, MAX_DECAY, 512).astype(np.float32)).astype(np.float64)

CONST_SPECS = [
    ("identb", [128, 128], BF16), ("identf", [128, 128], F32), ("onesf", [128, 128], F32),
    ("T128", [128, 256], BF16), ("T128b", [128, 256], BF16), ("F128c", [128, 256], F32),
    ("TWF2c", [128, 256], F32), ("TWF2s", [128, 256], F32),
    ("BDc", [128, 128], BF16), ("BDs", [128, 128], BF16), ("BDsn", [128, 128], BF16),
    ("RBD1", [128, 256], BF16), ("RBD2", [128, 256], BF16),
    ("T256lo", [128, 512], BF16), ("T256hi", [128, 512], BF16), ("T256c", [128, 512], BF16),
    ("TWH2c", [128, 512], F32), ("TWH2s", [128, 512], F32),
    ("TWT2c", [128, 2, 256], F32), ("TWT2s", [128, 2, 256], F32),
    ("ICc", [128, 2, 128], BF16), ("ISnc", [128, 2, 128], BF16),
    ("mnext", [128, 128], BF16), ("mprev", [128, 128], BF16),
    ("enext", [1, 128], BF16), ("eprev", [1, 128], BF16),
    ("ZcT", [33, 16384], F32), ("tcirc", [128, 2, 64], F32),
    ("iota128", [128, 128], F32), ("iota16", [128, 16], F32),
]

WEIGHT_SPECS = [
    ("x_b", [8192, 1024]), ("gmix", [1, 1024]), ("gffn", [1, 1024]), ("gfin", [1, 1024]),
    ("wa", [4, 128, 8, 384]), ("cw", [4, 3, 384]), ("cb", [4, 1, 384]), ("binh", [4, 1, 384]),
    ("wfT", [4, 128, 1024]), ("bfn", [4, 128, 1]),
    ("fw1", [33, 64]), ("fb1", [64, 1]), ("fw2", [64, 64]), ("fb2", [64, 1]), ("w3", [4, 64, 512]),
    ("skip", [4, 1, 256]),
    ("wg", [16, 128, 1024]), ("bg", [128, 16]), ("wy", [8, 128, 1024]), ("wo", [8, 128, 1024]),
    ("wq", [16, 128, 1024]), ("skT", [128, 2048]), ("ut", [128, 128, 1024]), ("pv", [128, 128, 1024]),
]


def build():
    cfg = CFG
    nc = bass.Bass("TRN2", target_bir_lowering=False)
    din = {}
    for name, shape, dt in CONST_SPECS:
        din[name] = nc.dram_tensor(name, shape, dt, kind="ExternalInput")
    for name, shape in WEIGHT_SPECS:
        din[name] = nc.dram_tensor(name, shape, F32, kind="ExternalInput")
    out_t = nc.dram_tensor("out", [2048, 1024], F32, kind="ExternalOutput")
    dbg = {}
    if cfg["DEBUG"]:
        dbg["u3"] = nc.dram_tensor("dbg_u3", [128, 384, 64], F32, kind="ExternalOutput")
        dbg["zf"] = nc.dram_tensor("dbg_zf", [128, 256, 64], F32, kind="ExternalOutput")
        dbg["zloc"] = nc.dram_tensor("dbg_zloc", [1024, 2048], F32, kind="ExternalOutput")
        dbg["kf"] = nc.dram_tensor("dbg_kf", [128, 2, 2, 512], F32, kind="ExternalOutput")
        dbg["xa"] = nc.dram_tensor("dbg_xa", [2048, 1024], F32, kind="ExternalOutput")
        dbg["mt"] = nc.dram_tensor("dbg_mt", [128, 2048], F32, kind="ExternalOutput")
    zloc = nc.dram_tensor("zloc", [1024, 2048], BF16, kind="Internal")
    WGs = nc.dram_tensor("WGs", [16, 128, 1024], BF16, kind="Internal")
    WYs = nc.dram_tensor("WYs", [8, 128, 1024], BF16, kind="Internal")
    WQs = nc.dram_tensor("WQs", [16, 128, 1024], BF16, kind="Internal")
    UTs = nc.dram_tensor("UTs", [128, 128, 1024], BF16, kind="Internal")
    Vs = nc.dram_tensor("Vs", [128, 128, 1024], BF16, kind="Internal")

    P = Prog(nc)
    with contextlib.ExitStack() as top:
        P.begin(top)

        uid = {"n": 0}

        def SB(stack, name, shape, dt=F32):
            uid["n"] += 1
            return stack.enter_context(nc.sbuf_tensor("sb%d_%s" % (uid["n"], name), shape, dt))

        PB = [top.enter_context(nc.psum_tensor("pb%d" % i, [128, 512], F32)) for i in range(8)]
        PT = PB[7][:].bitcast(BF16)

        identb = SB(top, "identb", [128, 128], BF16)
        identf = SB(top, "identf", [128, 128])
        epsT = SB(top, "epsT", [128, 1])
        onesb = SB(top, "onesb", [1, 128], BF16)
        P.dma("sp", identb[:], din["identb"].ap(), writes=["identb"])
        P.dma("sp", identf[:], din["identf"].ap(), writes=["identf"])
        P.op("dve", lambda e: e.memset(epsT[:], 1e-6), writes=["epsT"])
        P.op("dve", lambda e: e.memset(onesb[:], 1.0), writes=["onesb"])

        def prep_chunks():
            for i in range(16):
                yield din["wg"].ap()[i], WGs.ap()[i], ("WGs", i)
            for i in range(8):
                yield din["wy"].ap()[i], WYs.ap()[i], ("WYs", i)
            for i in range(16):
                yield din["wq"].ap()[i], WQs.ap()[i], ("WQs", i)
            for i in range(cfg["NCH"]):
                yield din["ut"].ap()[i], UTs.ap()[i], ("UTs", i)
                yield din["pv"].ap()[i], Vs.ap()[i], ("Vs", i)

        prep_state = {"it": prep_chunks(), "n": 0, "pend": None, "bufs": None}

        def emit_prep(n):
            p32, p16 = prep_state["bufs"]
            for _ in range(n):
                try:
                    src, dst, key = next(prep_state["it"])
                except StopIteration:
                    src = None
                if src is not None:
                    k = prep_state["n"]; prep_state["n"] += 1
                    P.dma("sp", p32[k % 2][:], src, writes=[("p32", k % 2)])
                if prep_state["pend"] is not None:
                    pdst, pkey, pk_ = prep_state["pend"]
                    P.dma("sp", pdst, p16[pk_ % 2][:], reads=[("p16", pk_ % 2)], writes=[pkey])
                    prep_state["pend"] = None
                if src is None:
                    return
                P.op("act", lambda e, k=k: e.copy(out=p16[k % 2][:], in_=p32[k % 2][:]), reads=[("p32", k % 2)], writes=[("p16", k % 2)])
                prep_state["pend"] = (dst, key, k)

        def rmsnorm_scale(xt, xkey, ss, rs, col, junk, gbc, hb, hkey):
            P.op("act", lambda e: e.activation(out=junk[:], in_=xt, func=ACT.Square, accum_out=ss[:, col:col + 1]),
                 reads=[xkey], writes=["junk", ("ss", col)])
            P.op("act", lambda e: e.activation(out=rs[:, col:col + 1], in_=ss[:, col:col + 1], func=ACT.Sqrt,
                                               scale=1.0 / 1024.0, bias=epsT[:]),
                 reads=[("ss", col), "epsT"], writes=[("rs", col)])
            P.op("dve", lambda e: e.reciprocal(out=rs[:, col:col + 1], in_=rs[:, col:col + 1]),
                 reads=[("rs", col)], writes=[("rs", col)])
            P.op("dve", lambda e: e.scalar_tensor_tensor(out=hb, in0=xt, scalar=rs[:, col:col + 1], in1=gbc[:],
                                                         op0=ALU.mult, op1=ALU.mult),
                 reads=[xkey, ("rs", col), "gbc"], writes=[hkey])

        if cfg["DO_MIX"]:
            with contextlib.ExitStack() as mx:
                cn = {}
                for name, shape, dt in CONST_SPECS:
                    if name in ("identb", "identf", "ZcT", "iota128", "iota16", "enext", "eprev", "mnext", "mprev"):
                        continue
                    cn[name] = SB(mx, "c_" + name, shape, dt)
                    P.dma("sp", cn[name][:], din[name].ap(), writes=["c_" + name])
                mnext = SB(mx, "mnext", [128, 128], BF16)
                mprev = SB(mx, "mprev", [128, 128], BF16)
                enext = SB(mx, "enext", [1, 128], BF16)
                eprev = SB(mx, "eprev", [1, 128], BF16)
                P.dma("sp", mnext[:], din["mnext"].ap(), writes=["mnext"])
                P.dma("sp", mprev[:], din["mprev"].ap(), writes=["mprev"])
                P.dma("sp", enext[:], din["enext"].ap(), writes=["enext"])
                P.dma("sp", eprev[:], din["eprev"].ap(), writes=["eprev"])
                gbc = SB(mx, "gbc_m", [128, 1024])
                P.dma("sp", gbc[:], DAP(din["gmix"], 0, [[0, 128], [1, 1024]]), writes=["gbc"])
                negpi = SB(mx, "negpi", [128, 1])
                P.op("dve", lambda e: e.memset(negpi[:], -math.pi), writes=["negpi"])

                hdn2T = SB(mx, "hdn2T", [64, 16384], BF16)
                with contextlib.ExitStack() as fs:
                    fw1 = SB(fs, "fw1", [33, 64]); fw2 = SB(fs, "fw2", [64, 64])
                    fb1 = SB(fs, "fb1", [64, 1]); fb2 = SB(fs, "fb2", [64, 1])
                    P.dma("sp", fw1[:], din["fw1"].ap(), writes=["fw1"])
                    P.dma("sp", fw2[:], din["fw2"].ap(), writes=["fw2"])
                    P.dma("sp", fb1[:], din["fb1"].ap(), writes=["fb1"])
                    P.dma("sp", fb2[:], din["fb2"].ap(), writes=["fb2"])
                    zc = [SB(fs, "zc%d" % i, [33, 2048]) for i in range(2)]
                    h1 = [SB(fs, "h1_%d" % i, [64, 512]) for i in range(2)]
                    w1t = [SB(fs, "w1t_%d" % i, [64, 512]) for i in range(2)]
                    w2t = [SB(fs, "w2t_%d" % i, [64, 512]) for i in range(2)]

                    def sin_layer(ps, pskey, bias, bkey, tmpa, tmpb, outap, outkeys, k):
                        ta, tb = tmpa[k % 2], tmpb[k % 2]
                        P.op("act", lambda e: e.activation(out=ta[:], in_=ps, func=ACT.Identity, bias=bias[:], scale=1.0),
                             reads=[pskey, bkey], writes=[("sa", k % 2)])
                        P.op("dve", lambda e: e.tensor_scalar(out=tb[:], in0=ta[:], scalar1=math.pi, scalar2=-2 * math.pi,
                                                              op0=ALU.is_gt, op1=ALU.mult),
                             reads=[("sa", k % 2)], writes=[("sb", k % 2)])
                        P.op("pool", lambda e: e.tensor_add(out=tb[:], in0=tb[:], in1=ta[:]),
                             reads=[("sa", k % 2), ("sb", k % 2)], writes=[("sb", k % 2)])
                        P.op("dve", lambda e: e.tensor_scalar(out=ta[:], in0=ta[:], scalar1=-math.pi, scalar2=2 * math.pi,
                                                              op0=ALU.is_lt, op1=ALU.mult),
                             reads=[("sa", k % 2)], writes=[("sa", k % 2)])
                        P.op("pool", lambda e: e.tensor_add(out=tb[:], in0=tb[:], in1=ta[:]),
                             reads=[("sa", k % 2), ("sb", k % 2)], writes=[("sb", k % 2)])
                        P.op("act", lambda e: e.activation(out=outap, in_=tb[:], func=ACT.Sin),
                             reads=[("sb", k % 2)], writes=outkeys)

                    k = 0
                    for ch in range(8):
                        zt = zc[ch % 2]
                        P.dma("sp", zt[:], DAP(din["ZcT"], ch * 2048, [[16384, 33], [1, 2048]]), writes=[("zc", ch % 2)])
                        for s in range(4):
                            psA = AP(PB[k % 2], 0, [[1, 512]], 0, 64)
                            P.op("pe", lambda e, psA=psA, zt=zt, s=s: e.matmul(out=psA, lhsT=fw1[:], rhs=zt[:, s * 512:(s + 1) * 512],
                                                                             start=True, stop=True),
                                 reads=["fw1", ("zc", ch % 2)], writes=[("pb", k % 2)])
                            sin_layer(psA, ("pb", k % 2), fb1, "fb1", w1t, w2t, h1[k % 2][:], [("h1", k % 2)], k)
                            psB = AP(PB[2 + k % 2], 0, [[1, 512]], 0, 64)
                            P.op("pe", lambda e, psB=psB, k=k: e.matmul(out=psB, lhsT=fw2[:], rhs=h1[k % 2][:], start=True, stop=True),
                                 reads=["fw2", ("h1", k % 2)], writes=[("pb", 2 + k % 2)])
                            col = ch * 2048 + s * 512
                            sin_layer(psB, ("pb", 2 + k % 2), fb2, "fb2", w1t, w2t, hdn2T[:, col:col + 512], ["hdn2T"], k)
                            k += 1
                    P.flush()

                U3 = SB(mx, "U3", [128, 384, 64], BF16)

                for gi in range(cfg["NG"]):
                    zf_stack = contextlib.ExitStack()
                    ZF = SB(zf_stack, "ZF", [128, 256, 64], BF16)
                    with contextlib.ExitStack() as pa:
                        WA3 = SB(pa, "WA3", [128, 3, 8, 384], BF16)
                        WF = SB(pa, "WF", [128, 8, 256], BF16)
                        brow_h = SB(pa, "brow_h", [1, 384], BF16)
                        e0row = SB(pa, "e0row", [1, 384], BF16)
                        e2row = SB(pa, "e2row", [1, 384], BF16)
                        brow_f = SB(pa, "brow_f", [1, 256], BF16)
                        pp = contextlib.ExitStack()
                        wa_sb = SB(pp, "wa_sb", [128, 8, 384])
                        cwb = SB(pp, "cwb", [128, 3, 384])
                        wfT = SB(pp, "wfT", [128, 1024])
                        bfn = SB(pp, "bfn", [128, 1])
                        rows = SB(pp, "rows", [1, 5, 384])
                        rtmp = SB(pp, "rtmp", [1, 384])
                        P.dma("sp", wa_sb[:], din["wa"].ap()[gi], writes=["wa_sb"])
                        P.dma("sp", cwb[:], DAP(din["cw"], gi * 1152, [[0, 128], [384, 3], [1, 384]]), writes=["cwb"])
                        P.dma("sp", wfT[:], din["wfT"].ap()[gi], writes=["wfT"])
                        P.dma("sp", bfn[:], din["bfn"].ap()[gi], writes=["bfn"])
                        P.dma("sp", rows[0:1, 0, :], din["cb"].ap()[gi], writes=["rows0"])
                        P.dma("sp", rows[0:1, 1, :], din["binh"].ap()[gi], writes=["rows1"])
                        P.dma("sp", rows[0:1, 2:5, :], DAP(din["cw"], gi * 1152, [[0, 1], [384, 3], [1, 384]]), writes=["rows2"])
                        for j in range(3):
                            eng = ("dve", "pool", "dve")[j]
                            P.op(eng, lambda e, j=j: e.tensor_tensor(out=WA3[:, j, :, :], in0=wa_sb[:],
                                                                      in1=AP(cwb, j * 384, [[0, 8], [1, 384]]), op=ALU.mult),
                                 reads=["wa_sb", "cwb"], writes=[("WA3", j)])
                        for dc in range(8):
                            P.op("pe", lambda e, dc=dc: e.matmul(out=PB[4 + dc % 2][:, 0:256], lhsT=wfT[:, dc * 128:(dc + 1) * 128],
                                                                 rhs=cn["F128c"][:], start=True, stop=True),
                                 reads=["wfT", "c_F128c"], writes=[("pb", 4 + dc % 2)])
                            P.op("act", lambda e, dc=dc: e.copy(out=WF[:, dc, :], in_=PB[4 + dc % 2][:, 0:256]),
                                 reads=[("pb", 4 + dc % 2)], writes=[("WF", dc)])
                        P.op("pe", lambda e: e.matmul(out=PB[6][0:1, 0:256], lhsT=bfn[:, 0:1], rhs=cn["F128c"][:], start=True, stop=True),
                             reads=["bfn", "c_F128c"], writes=[("pb", 6)])
                        P.op("act", lambda e: e.copy(out=brow_f[:], in_=PB[6][0:1, 0:256]), reads=[("pb", 6)], writes=["brow_f"])
                        P.op("dve", lambda e: e.tensor_add(out=rtmp[:], in0=rows[0:1, 2, :], in1=rows[0:1, 3, :]), reads=["rows0", "rows1", "rows2"], writes=["rtmp"])
                        P.op("dve", lambda e: e.tensor_add(out=rtmp[:], in0=rtmp[:], in1=rows[0:1, 4, :]), reads=["rows0", "rows1", "rows2", "rtmp"], writes=["rtmp"])
                        P.op("dve", lambda e: e.tensor_mul(out=rtmp[:], in0=rtmp[:], in1=rows[0:1, 1, :]), reads=["rows0", "rows1", "rows2", "rtmp"], writes=["rtmp"])
                        P.op("dve", lambda e: e.tensor_add(out=brow_h[:], in0=rtmp[:], in1=rows[0:1, 0, :]), reads=["rows0", "rows1", "rows2", "rtmp"], writes=["brow_h"])
                        P.op("dve", lambda e: e.scalar_tensor_tensor(out=e0row[:], in0=rows[0:1, 1, :], scalar=-1.0, in1=rows[0:1, 2, :],
                                                                     op0=ALU.mult, op1=ALU.mult), reads=["rows0", "rows1", "rows2"], writes=["e0row"])
                        P.op("dve", lambda e: e.scalar_tensor_tensor(out=e2row[:], in0=rows[0:1, 1, :], scalar=-1.0, in1=rows[0:1, 4, :],
                                                                     op0=ALU.mult, op1=ALU.mult), reads=["rows0", "rows1", "rows2"], writes=["e2row"])

                        P.flush()
                        pp.close()
                        xt = [SB(pa, "xt%d" % i, [128, 1024]) for i in range(2)]
                        junk = SB(pa, "junk", [128, 1024], BF16)
                        hb = [SB(pa, "hb%d" % i, [128, 1024], BF16) for i in range(2)]
                        ss = SB(pa, "ss", [128, 2]); rs = SB(pa, "rs", [128, 2])
                        hTr = [SB(pa, "hTr%d" % i, [128, 8, 128], BF16) for i in range(4)]
                        hT0 = SB(pa, "hT0", [128, 8, 128], BF16)
                        hT63 = SB(pa, "hT63", [128, 8, 128], BF16)
                        hTm1 = SB(pa, "hTm1", [128, 8, 128], BF16)
                        hT64 = SB(pa, "hT64", [128, 8, 128], BF16)

                        slot = {}

                        def produce(a, k):
                            x_ap = DAP(din["x_b"], a * 1024, [[64 * 1024, 128], [1, 1024]])
                            P.dma("sp", xt[k % 2][:], x_ap, writes=[("xt", k % 2)])
                            rmsnorm_scale(xt[k % 2][:], ("xt", k % 2), ss, rs, k % 2, junk, gbc, hb[k % 2][:], ("hb", k % 2))
                            for dc in range(8):
                                P.op("pe", lambda e, dc=dc, k=k: e.transpose(out=PT[:, dc * 128:(dc + 1) * 128],
                                                                             in_=hb[k % 2][:, dc * 128:(dc + 1) * 128], identity=identb[:]),
                                     reads=[("hb", k % 2), "identb"], writes=["PT"])
                            if a == 0:
                                dst, key = hT0, "hT0"
                            elif a == 63:
                                dst, key = hT63, "hT63"
                            else:
                                dst, key = hTr[a % 4], ("hTr", a % 4)
                            slot[a] = (dst, key)
                            P.op("act", lambda e, dst=dst: e.copy(out=AP(dst, 0, [[1, 1024]]), in_=PT[:]), reads=["PT"], writes=[key])

                        def mm(a, k):
                            ph = PB[k % 2]; pf = PB[2 + k % 2]
                            srcs = [slot[a - 1] if a > 0 else (hTm1, "hTm1"), slot[a], slot[a + 1] if a < 63 else (hT64, "hT64")]
                            first = True
                            for j in range(3):
                                src, skey = srcs[j]
                                for dc in range(8):
                                    P.op("pe", lambda e, src=src, j=j, dc=dc, first=first, ph=ph: e.matmul(
                                        out=ph[:, 0:384], lhsT=src[:, dc, :], rhs=WA3[:, j, dc, :], start=first, stop=False),
                                        reads=[skey, ("WA3", j)], writes=[("pb", k % 2)])
                                    first = False
                            last = a not in (0, 63)
                            P.op("pe", lambda e, ph=ph, last=last: e.matmul(out=ph[:, 0:384], lhsT=onesb[:], rhs=brow_h[:], start=False, stop=last),
                                 reads=["onesb", "brow_h"], writes=[("pb", k % 2)])
                            if a == 0:
                                P.op("pe", lambda e, ph=ph: e.matmul(out=ph[:, 0:384], lhsT=eprev[:], rhs=e0row[:], start=False, stop=True),
                                     reads=["eprev", "e0row"], writes=[("pb", k % 2)])
                            if a == 63:
                                P.op("pe", lambda e, ph=ph: e.matmul(out=ph[:, 0:384], lhsT=enext[:], rhs=e2row[:], start=False, stop=True),
                                     reads=["enext", "e2row"], writes=[("pb", k % 2)])
                            src, skey = slot[a]
                            for dc in range(8):
                                P.op("pe", lambda e, src=src, dc=dc, pf=pf: e.matmul(out=pf[:, 0:256], lhsT=src[:, dc, :], rhs=WF[:, dc, :],
                                                                                    start=(dc == 0), stop=False),
                                     reads=[skey, ("WF", dc)], writes=[("pb", 2 + k % 2)])
                            P.op("pe", lambda e, pf=pf: e.matmul(out=pf[:, 0:256], lhsT=onesb[:], rhs=brow_f[:], start=False, stop=True),
                                 reads=["onesb", "brow_f"], writes=[("pb", 2 + k % 2)])
                            P.op("dve", lambda e, ph=ph, a=a: e.tensor_copy(out=AP(U3, a, [[64, 384]]), in_=ph[:, 0:384]),
                                 reads=[("pb", k % 2)], writes=["U3"])
                            P.op("act", lambda e, pf=pf, a=a: e.copy(out=AP(ZF, a, [[64, 256]]), in_=pf[:, 0:256]),
                                 reads=[("pb", 2 + k % 2)], writes=["ZF"])

                        produce(63, 0)
                        P.op("dve", lambda e: e.tensor_tensor(out=hTm1[:, :, 1:128], in0=hT63[:, :, 0:127],
                                                              in1=AP(mprev, 1, [[0, 8], [1, 127]]), op=ALU.mult),
                             reads=["hT63", "mprev"], writes=["hTm1"])
                        P.op("dve", lambda e: e.tensor_tensor(out=hTm1[:, :, 0:1], in0=hT63[:, :, 127:128],
                                                              in1=AP(mprev, 0, [[0, 8], [1, 1]]), op=ALU.mult),
                             reads=["hT63", "mprev"], writes=["hTm1"])
                        produce(0, 1)
                        P.op("dve", lambda e: e.tensor_tensor(out=hT64[:, :, 0:127], in0=hT0[:, :, 1:128],
                                                              in1=AP(mnext, 0, [[0, 8], [1, 127]]), op=ALU.mult),
                             reads=["hT0", "mnext"], writes=["hT64"])
                        P.op("dve", lambda e: e.tensor_tensor(out=hT64[:, :, 127:128], in0=hT0[:, :, 0:1],
                                                              in1=AP(mnext, 127, [[0, 8], [1, 1]]), op=ALU.mult),
                             reads=["hT0", "mnext"], writes=["hT64"])
                        kk = 2
                        for a in range(1, 63):
                            produce(a, kk); kk += 1
                            mm(a - 1, a - 1)
                        mm(62, 62)
                        mm(63, 63)
                        if cfg["DEBUG"] and gi == 0 and cfg.get("DUMP_A", False):
                            with contextlib.ExitStack() as ds:
                                d32 = SB(ds, "d32", [128, 64 * 64])
                                for cch in range(6):
                                    P.op("dve", lambda e, cch=cch: e.tensor_copy(out=d32[:], in_=AP(U3, cch * 4096, [[1, 4096]])), reads=["U3"], writes=["d32"])
                                    P.dma("sp", DAP(dbg["u3"], cch * 4096, [[384 * 64, 128], [1, 4096]]), d32[:], reads=["d32"])
                                for cch in range(4):
                                    P.op("dve", lambda e, cch=cch: e.tensor_copy(out=d32[:], in_=AP(ZF, cch * 4096, [[1, 4096]])), reads=["ZF"], writes=["d32"])
                                    P.dma("sp", DAP(dbg["zf"], cch * 4096, [[256 * 64, 128], [1, 4096]]), d32[:], reads=["d32"])
                                P.flush()
                        P.flush()

                    with contextlib.ExitStack() as pf_:
                        ZFN = SB(pf_, "ZFN", [128, 64, 128], BF16)
                        ta_ = [SB(pf_, "fta%d" % i, [128, 256]) for i in range(2)]
                        tb_ = [SB(pf_, "ftb%d" % i, [128, 256]) for i in range(2)]
                        Bre = [SB(pf_, "fBre%d" % i, [128, 4, 128], BF16) for i in range(2)]
                        Bim = [SB(pf_, "fBim%d" % i, [128, 4, 128], BF16) for i in range(2)]
                        k = 0
                        for jb in range(16):
                            for jj in range(4):
                                j = 4 * jb + jj
                                pA = PB[k % 2]
                                P.op("pe", lambda e, pA=pA, j=j: e.matmul(out=pA[:, 0:256], lhsT=AP(ZF, 2 * j * 64, [[1, 128]]), rhs=cn["T128"][:],
                                                                           start=True, stop=False),
                                     reads=["ZF", "c_T128"], writes=[("pb", k % 2)])
                                P.op("pe", lambda e, pA=pA, j=j: e.matmul(out=pA[:, 0:256], lhsT=AP(ZF, (128 + 2 * j) * 64, [[1, 128]]), rhs=cn["T128b"][:],
                                                                           start=False, stop=True),
                                     reads=["ZF", "c_T128b"], writes=[("pb", k % 2)])
                                P.op("dve", lambda e, pA=pA, k=k: e.tensor_tensor(out=ta_[k % 2][:], in0=pA[:, 0:256], in1=cn["TWF2c"][:], op=ALU.mult),
                                     reads=[("pb", k % 2), "c_TWF2c"], writes=[("fta", k % 2)])
                                P.op("dve", lambda e, pA=pA, k=k: e.tensor_tensor(out=tb_[k % 2][:], in0=pA[:, 0:256], in1=cn["TWF2s"][:], op=ALU.mult),
                                     reads=[("pb", k % 2), "c_TWF2s"], writes=[("ftb", k % 2)])
                                P.op("pool", lambda e, k=k, jb=jb, jj=jj: e.tensor_add(out=Bre[jb % 2][:, jj, :], in0=ta_[k % 2][:, 0:128], in1=tb_[k % 2][:, 128:256]),
                                     reads=[("fta", k % 2), ("ftb", k % 2)], writes=[("fBre", jb % 2)])
                                P.op("pool", lambda e, k=k, jb=jb, jj=jj: e.tensor_sub(out=Bim[jb % 2][:, jj, :], in0=ta_[k % 2][:, 128:256], in1=tb_[k % 2][:, 0:128]),
                                     reads=[("fta", k % 2), ("ftb", k % 2)], writes=[("fBim", jb % 2)])
                                k += 1
                            pY = PB[2 + jb % 2]
                            P.op("pe", lambda e, pY=pY, jb=jb: e.matmul(out=pY[:], lhsT=cn["BDc"][:], rhs=AP(Bre[jb % 2], 0, [[1, 512]]), start=True, stop=False),
                                 reads=["c_BDc", ("fBre", jb % 2)], writes=[("pb", 2 + jb % 2)])
                            P.op("pe", lambda e, pY=pY, jb=jb: e.matmul(out=pY[:], lhsT=cn["BDs"][:], rhs=AP(Bim[jb % 2], 0, [[1, 512]]), start=False, stop=True),
                                 reads=["c_BDs", ("fBim", jb % 2)], writes=[("pb", 2 + jb % 2)])
                            P.op("act", lambda e, pY=pY, jb=jb: e.activation(out=AP(ZFN, 4 * jb * 128, [[1, 512]]), in_=pY[:], func=ACT.Identity, scale=1.0 / 1024.0),
                                 reads=[("pb", 2 + jb % 2)], writes=["ZFN"])
                        for c2 in range(2):
                            dst = DAP(zloc, (512 + gi * 128 + c2) * 2048, [[128, 16], [2 * 2048, 64], [1, 128]])
                            P.dma("sp", dst, AP(ZFN, 0, [[128, 64], [1, 128]], c2 * 64, 16), reads=["ZFN"], writes=[("zloc", "fn", gi)])
                        P.flush()

                    zf_stack.close()
                    with contextlib.ExitStack() as ph_:
                        ZHY = SB(ph_, "ZHY", [128, 128, 64], BF16)
                        W3g = SB(ph_, "W3g", [64, 512], BF16)
                        skb = SB(ph_, "skb", [128, 256])
                        W3s = SB(ph_, "W3s", [64, 512])
                        P.dma("sp", W3s[:], din["w3"].ap()[gi], writes=["W3s"])
                        P.op("act", lambda e: e.copy(out=W3g[:], in_=W3s[:]), reads=["W3s"], writes=["W3g"])
                        P.dma("sp", skb[:], DAP(din["skip"], gi * 256, [[0, 128], [1, 256]]), writes=["skb"])
                        NL = 3
                        if gi == 0:
                            prep_state["bufs"] = ([SB(ph_, "p32_%d" % i, [128, 1024]) for i in range(2)],
                                                  [SB(ph_, "p16_%d" % i, [128, 1024], BF16) for i in range(2)])
                        DWp = [SB(ph_, "DWp%d" % i, [128, 2, 2, 64]) for i in range(NL)]
                        KC = [SB(ph_, "KC%d" % i, [128, 2, 2, 2, 64], BF16) for i in range(NL)]
                        l1p = [SB(ph_, "l1p%d" % i, [128, 8]) for i in range(NL)]
                        rn = [SB(ph_, "rn%d" % i, [128, 4]) for i in range(NL)]
                        rncol = [SB(ph_, "rncol%d" % i, [128, 2]) for i in range(NL)]
                        ta = [[SB(ph_, "hta%d_%d" % (l, i), [128, 512]) for i in range(2)] for l in range(NL)]
                        tb = [[SB(ph_, "htb%d_%d" % (l, i), [128, 512]) for i in range(2)] for l in range(NL)]
                        Bre = [[SB(ph_, "hBre%d_%d" % (l, i), [128, 256], BF16) for i in range(2)] for l in range(NL)]
                        Bim = [[SB(ph_, "hBim%d_%d" % (l, i), [128, 256], BF16) for i in range(2)] for l in range(NL)]
                        K2 = [SB(ph_, "K2_%d" % i, [128, 2, 2, 512]) for i in range(NL)]
                        Yre = [SB(ph_, "Yre%d" % i, [128, 256], BF16) for i in range(NL)]
                        Yim = [SB(ph_, "Yim%d" % i, [128, 256], BF16) for i in range(NL)]
                        Ere = [SB(ph_, "Ere%d" % i, [128, 2, 128], BF16) for i in range(NL)]
                        Eim = [SB(ph_, "Eim%d" % i, [128, 2, 128], BF16) for i in range(NL)]
                        t1 = [SB(ph_, "gt1_%d" % i, [128, 128]) for i in range(NL)]
                        t2 = [SB(ph_, "gt2_%d" % i, [128, 128]) for i in range(NL)]
                        z1 = [SB(ph_, "z1_%d" % i, [128, 128], BF16) for i in range(NL)]
                        cnt = [0] * NL
                        last_flush = [P.ninstr]
                        PYB = [PB[1], PB[3], PB[5]]

                        def fwd_fft(ln, lhs_list, lkeys, tabs, tkeys):
                            k = cnt[ln]; cnt[ln] += 1
                            kb = k % 2
                            pA = PB[2 * ln]; pakey = ("pbA", ln)
                            pX = PB[2 * ln + 1]; pxkey = ("pbX", ln)
                            n = len(lhs_list)
                            for i in range(n):
                                P.op("pe", lambda e, i=i, n=n: e.matmul(out=pA[:], lhsT=lhs_list[i], rhs=tabs[i][:], start=(i == 0), stop=(i == n - 1)),
                                     reads=list(lkeys) + [tkeys[i]], writes=[pakey])
                            yield
                            P.op("dve", lambda e: e.tensor_tensor(out=ta[ln][kb][:], in0=pA[:], in1=cn["TWH2c"][:], op=ALU.mult),
                                 reads=[pakey, "c_TWH2c"], writes=[("hta", ln, kb)])
                            P.op("dve", lambda e: e.tensor_tensor(out=tb[ln][kb][:], in0=pA[:], in1=cn["TWH2s"][:], op=ALU.mult),
                                 reads=[pakey, "c_TWH2s"], writes=[("htb", ln, kb)])
                            yield
                            P.op("pool", lambda e: e.tensor_add(out=Bre[ln][kb][:], in0=ta[ln][kb][:, 0:256], in1=tb[ln][kb][:, 256:512]),
                                 reads=[("hta", ln, kb), ("htb", ln, kb)], writes=[("hBre", ln, kb)])
                            P.op("pool", lambda e: e.tensor_sub(out=Bim[ln][kb][:], in0=ta[ln][kb][:, 256:512], in1=tb[ln][kb][:, 0:256]),
                                 reads=[("hta", ln, kb), ("htb", ln, kb)], writes=[("hBim", ln, kb)])
                            yield
                            seq = [(0, "BDc", Bre, "hBre", True, False), (0, "BDs", Bim, "hBim", False, True),
                                   (256, "BDc", Bim, "hBim", True, False), (256, "BDsn", Bre, "hBre", False, True)]
                            for (off, tn, src, sk, st, sp_) in seq:
                                P.op("pe", lambda e, off=off, tn=tn, src=src, st=st, sp_=sp_: e.matmul(
                                    out=pX[:, off:off + 256], lhsT=cn[tn][:], rhs=src[ln][kb][:], start=st, stop=sp_),
                                    reads=["c_" + tn, (sk, ln, kb)], writes=[pxkey])
                            yield

                        def pair_gen(j, ln, jj):
                            pA = PB[2 * ln]; pakey = ("pbA", ln)
                            pX = PB[2 * ln + 1]; pxkey = ("pbX", ln)
                            pYk = pxkey; pNk = pxkey
                            for c2 in range(2):
                                cglob = gi * 128 + 2 * j + c2
                                P.op("act", lambda e, c2=c2, cglob=cglob: e.activation(
                                    out=AP(DWp[ln], c2 * 64, [[128, 2], [1, 64]]), in_=cn["tcirc"][:], func=ACT.Exp, scale=-float(DELTAS[cglob])),
                                    reads=["c_tcirc"], writes=[("DWp", ln)])
                            for chn in range(2):
                                P.op("dve", lambda e, chn=chn: e.tensor_tensor(
                                    out=AP(KC[ln], chn * 256, [[128, 2], [64, 2], [1, 64]]),
                                    in0=AP(PB[6 + chn], 2 * jj, [[4, 2], [1, 2], [8, 64]]),
                                    in1=AP(DWp[ln], chn * 128, [[0, 2], [64, 2], [1, 64]]), op=ALU.mult),
                                    reads=[("pb", 6 + chn), ("DWp", ln)], writes=[("KC", ln)])
                            yield "evac"
                            P.op("dve", lambda e: e.tensor_reduce(out=l1p[ln][:], in_=AP(KC[ln], 0, [[64, 8], [1, 64]]), axis=AX.X, op=ALU.add,
                                                                  apply_absolute_value=True),
                                 reads=[("KC", ln)], writes=[("l1p", ln)])
                            P.op("pe", lambda e: e.matmul(out=PYB[ln][:, 256:264], lhsT=cn["onesf"][:], rhs=l1p[ln][:], start=True, stop=True),
                                 reads=["c_onesf", ("l1p", ln)], writes=[pNk])
                            yield
                            P.op("dve", lambda e: e.tensor_copy(out=rn[ln][:], in_=PYB[ln][:, 256:260]), reads=[pNk], writes=[("rn", ln)])
                            P.op("dve", lambda e: e.tensor_add(out=rn[ln][:], in0=rn[ln][:], in1=PYB[ln][:, 260:264]), reads=[pNk, ("rn", ln)], writes=[("rn", ln)])
                            P.op("dve", lambda e: e.reciprocal(out=rn[ln][:], in_=rn[ln][:]), reads=[("rn", ln)], writes=[("rn", ln)])
                            P.op("pool", lambda e: e.tensor_copy(out=rncol[ln][0:64, :], in_=AP(rn[ln], 0, [[2, 2]], 0, 64)), reads=[("rn", ln)], writes=[("rncol", ln)])
                            P.op("pool", lambda e: e.tensor_copy(out=rncol[ln][64:128, :], in_=AP(rn[ln], 1, [[2, 2]], 64, 64)), reads=[("rn", ln)], writes=[("rncol", ln)])
                            yield
                            for o in range(2):
                                yield from fwd_fft(ln, [AP(KC[ln], o * 128, [[1, 128]]), AP(KC[ln], 256 + o * 128, [[1, 128]])], [("KC", ln)],
                                                   [cn["T256lo"], cn["T256hi"]], ["c_T256lo", "c_T256hi"])
                                for ri in range(2):
                                    P.op("act", lambda e, o=o, ri=ri: e.activation(
                                        out=AP(K2[ln], (o * 2 + ri) * 512, [[256, 2], [1, 256]]),
                                        in_=AP(pX, ri * 256, [[0, 2], [1, 256]]), func=ACT.Identity, scale=rncol[ln][:, o:o + 1]),
                                        reads=[pxkey, ("rncol", ln)], writes=[("K2", ln)])
                                yield
                            if cfg["DEBUG"] and gi == 0 and j == 0:
                                P.dma("sp", DAP(dbg["kf"], 0, [[2048, 128], [1, 2048]]), AP(K2[0], 0, [[1, 2048]]), reads=[("K2", 0)])
                            for o in range(2):
                                if o == 0:
                                    lhs = AP(U3, 2 * j * 64, [[1, 128]]); lk = "U3"
                                else:
                                    lhs = z1[ln][:]; lk = ("z1", ln)
                                yield from fwd_fft(ln, [lhs], [lk], [cn["T256c"]], ["c_T256c"])
                                k = cnt[ln]; cnt[ln] += 1
                                kb = k % 2
                                P.op("dve", lambda e, o=o, kb=kb: e.tensor_tensor(out=ta[ln][kb][:], in0=pX[:], in1=AP(K2[ln], (o * 2) * 512, [[1, 512]]), op=ALU.mult),
                                     reads=[pxkey, ("K2", ln)], writes=[("hta", ln, kb)])
                                P.op("dve", lambda e, o=o, kb=kb: e.tensor_tensor(out=tb[ln][kb][:], in0=pX[:], in1=AP(K2[ln], (o * 2 + 1) * 512, [[1, 512]]), op=ALU.mult),
                                     reads=[pxkey, ("K2", ln)], writes=[("htb", ln, kb)])
                                yield
                                P.op("pool", lambda e, kb=kb: e.tensor_sub(out=Yre[ln][:], in0=ta[ln][kb][:, 0:256], in1=tb[ln][kb][:, 256:512]),
                                     reads=[("hta", ln, kb), ("htb", ln, kb)], writes=[("Yre", ln)])
                                P.op("pool", lambda e, kb=kb: e.tensor_add(out=Yim[ln][:], in0=tb[ln][kb][:, 0:256], in1=ta[ln][kb][:, 256:512]),
                                     reads=[("hta", ln, kb), ("htb", ln, kb)], writes=[("Yim", ln)])
                                yield
                                pD = pA
                                for half in range(2):
                                    P.op("pe", lambda e, half=half: e.matmul(out=pD[:, half * 256:(half + 1) * 256], lhsT=Yre[ln][:, half * 128:(half + 1) * 128],
                                                                             rhs=cn["RBD1"][:], start=True, stop=False),
                                         reads=[("Yre", ln), "c_RBD1"], writes=[pakey])
                                    P.op("pe", lambda e, half=half: e.matmul(out=pD[:, half * 256:(half + 1) * 256], lhsT=Yim[ln][:, half * 128:(half + 1) * 128],
                                                                             rhs=cn["RBD2"][:], start=False, stop=True),
                                         reads=[("Yim", ln), "c_RBD2"], writes=[pakey])
                                yield
                                k = cnt[ln]; cnt[ln] += 1
                                kb = k % 2
                                P.op("dve", lambda e, kb=kb: e.tensor_tensor(out=ta[ln][kb][:], in0=pD[:], in1=AP(cn["TWT2c"], 0, [[1, 512]]), op=ALU.mult),
                                     reads=[pakey, "c_TWT2c"], writes=[("hta", ln, kb)])
                                P.op("dve", lambda e, kb=kb: e.tensor_tensor(out=tb[ln][kb][:], in0=pD[:], in1=AP(cn["TWT2s"], 0, [[1, 512]]), op=ALU.mult),
                                     reads=[pakey, "c_TWT2s"], writes=[("htb", ln, kb)])
                                yield
                                P.op("pool", lambda e, kb=kb: e.tensor_sub(out=Ere[ln][:], in0=AP(ta[ln][kb], 0, [[256, 2], [1, 128]]), in1=AP(tb[ln][kb], 128, [[256, 2], [1, 128]])),
                                     reads=[("hta", ln, kb), ("htb", ln, kb)], writes=[("Ere", ln)])
                                P.op("pool", lambda e, kb=kb: e.tensor_add(out=Eim[ln][:], in0=AP(ta[ln][kb], 128, [[256, 2], [1, 128]]), in1=AP(tb[ln][kb], 0, [[256, 2], [1, 128]])),
                                     reads=[("hta", ln, kb), ("htb", ln, kb)], writes=[("Eim", ln)])
                                yield
                                M = 128 if o == 0 else 32
                                pY = PYB[ln]
                                seq = [("ICc", Ere, "Ere", 0), ("ICc", Ere, "Ere", 1), ("ISnc", Eim, "Eim", 0), ("ISnc", Eim, "Eim", 1)]
                                for i, (tn, src, sk, half) in enumerate(seq):
                                    P.op("pe", lambda e, tn=tn, src=src, half=half, i=i, M=M: e.matmul(
                                        out=pY[0:M, 0:128], lhsT=cn[tn][:, half, 0:M], rhs=src[ln][:, half, :], start=(i == 0), stop=(i == 3)),
                                        reads=["c_" + tn, (sk, ln)], writes=[pYk])
                                yield
                                if o == 0:
                                    P.op("pool", lambda e: e.tensor_tensor(out=AP(t1[ln], 0, [[64, 2], [1, 64]]), in0=AP(U3, 2 * j * 64, [[64, 2], [1, 64]]),
                                                                          in1=AP(skb, 2 * j, [[1, 2], [0, 64]]), op=ALU.mult),
                                         reads=["U3", "skb"], writes=[("gt1", ln)])
                                    P.op("dve", lambda e: e.tensor_add(out=t2[ln][:], in0=pY[:, 0:128], in1=t1[ln][:]), reads=[pYk, ("gt1", ln)], writes=[("gt2", ln)])
                                    P.op("pool", lambda e: e.tensor_mul(out=z1[ln][:], in0=t2[ln][:], in1=AP(U3, (128 + 2 * j) * 64, [[1, 128]])),
                                         reads=[("gt2", ln), "U3"], writes=[("z1", ln)])
                                else:
                                    P.op("pool", lambda e: e.tensor_tensor(out=AP(t1[ln], 0, [[64, 2], [1, 64]], 0, 32), in0=AP(z1[ln], 0, [[64, 2], [1, 64]], 0, 32),
                                                                          in1=AP(skb, 128 + 2 * j, [[1, 2], [0, 64]], 0, 32), op=ALU.mult),
                                         reads=[("z1", ln), "skb"], writes=[("gt1", ln)])
                                    P.op("dve", lambda e: e.tensor_add(out=t2[ln][0:32, :], in0=pY[0:32, 0:128], in1=t1[ln][0:32, :]), reads=[pYk, ("gt1", ln)], writes=[("gt2", ln)])
                                    P.op("pool", lambda e: e.tensor_mul(out=AP(ZHY, 2 * j * 64, [[1, 128]], 0, 32), in0=t2[ln][0:32, :],
                                                                      in1=AP(U3, (256 + 2 * j) * 64, [[1, 128]], 0, 32)),
                                         reads=[("gt2", ln), "U3"], writes=["ZHY"])
                                yield

                        def taps(jp):
                            for chn in range(2):
                                for n2 in range(64):
                                    P.op("pe", lambda e, chn=chn, n2=n2, jp=jp: e.matmul(
                                        out=PB[6 + chn][:, n2 * 8:(n2 + 1) * 8],
                                        lhsT=hdn2T[:, n2 * 256 + chn * 128: n2 * 256 + chn * 128 + 128],
                                        rhs=AP(W3g, chn * 128 + 4 * jp, [[256, 2], [1, 4]], 0, 64), start=True, stop=True),
                                        reads=["hdn2T", "W3g"], writes=[("pb", 6 + chn)])

                        npair = cfg["NPAIR"]
                        free_lanes = list(range(NL))
                        active = []
                        nextj = 0
                        while nextj < npair or active:
                            while free_lanes and nextj < npair:
                                j = nextj; nextj += 1
                                if j % 2 == 0:
                                    taps(j // 2)
                                    if gi == 0:
                                        emit_prep(10)
                                ln = free_lanes.pop(0)
                                g_ = pair_gen(j, ln, j % 2)
                                next(g_)
                                active.append((g_, ln))
                            for (g_, ln) in list(active):
                                try:
                                    next(g_)
                                except StopIteration:
                                    active.remove((g_, ln))
                                    free_lanes.append(ln)
                            if P.ninstr - last_flush[0] > 6000:
                                P.flush(); last_flush[0] = P.ninstr
                        if gi == 0:
                            emit_prep(400)
                        for cq in range(4):
                            dst = DAP(zloc, (gi * 128 + cq * 32) * 2048, [[64, 32], [2048, 32], [1, 64]])
                            P.dma("sp", dst, AP(ZHY, cq * 32 * 64, [[64, 32], [1, 64]], 0, 32), reads=["ZHY"], writes=[("zloc", "hy", gi, cq)])
                        P.flush()

        if cfg["DEBUG"]:
            with contextlib.ExitStack() as ds:
                zb = SB(ds, "dzb", [128, 8, 2048], BF16)
                zf32 = SB(ds, "dzf", [128, 8, 2048])
                rk = [("zloc", "fn", gi) for gi in range(cfg["NG"])] + [("zloc", "hy", gi, cq) for gi in range(cfg["NG"]) for cq in range(4)]
                P.dma("sp", zb[:], DAP(zloc, 0, [[2048, 128], [128 * 2048, 8], [1, 2048]]), reads=rk, writes=["dzb"])
                P.op("dve", lambda e: e.tensor_copy(out=zf32[:], in_=zb[:]), reads=["dzb"], writes=["dzf"])
                P.dma("sp", DAP(dbg["zloc"], 0, [[2048, 128], [128 * 2048, 8], [1, 2048]]), zf32[:], reads=["dzf"])
                P.flush()

        if cfg["DO_C"]:
            with contextlib.ExitStack() as cs:
                gbc = SB(cs, "gbc_c", [128, 1024])
                gbf = SB(cs, "gbf_c", [128, 1024])
                gfin = SB(cs, "gfin_c", [128, 1024])
                P.dma("sp", gbc[:], DAP(din["gmix"], 0, [[0, 128], [1, 1024]]), reads=[], writes=["gbc"])
                P.dma("sp", gbf[:], DAP(din["gffn"], 0, [[0, 128], [1, 1024]]), writes=["gbf"])
                P.dma("sp", gfin[:], DAP(din["gfin"], 0, [[0, 128], [1, 1024]]), writes=["gfin"])
                bg = SB(cs, "bg", [128, 16])
                P.dma("sp", bg[:], din["bg"].ap(), writes=["bg"])
                WO = SB(cs, "WO", [128, 8, 1024], BF16)
                SKT = SB(cs, "SKT", [128, 2048], BF16)
                iota128 = SB(cs, "iota128", [128, 128]); iota16 = SB(cs, "iota16", [128, 16])
                P.dma("sp", iota128[:], din["iota128"].ap(), writes=["iota128"])
                P.dma("sp", iota16[:], din["iota16"].ap(), writes=["iota16"])

                xres = SB(cs, "xres", [128, 2, 1024])
                junk = SB(cs, "junkc", [128, 1024], BF16)
                hbm = SB(cs, "hbm", [128, 1024], BF16)
                ss = SB(cs, "ssc", [128, 2]); rs = SB(cs, "rsc", [128, 2])
                hT = SB(cs, "hTc", [128, 8, 256], BF16)
                zT = SB(cs, "zT", [128, 8, 256], BF16)
                wst = [SB(cs, "wst%d" % i, [128, 1024], BF16) for i in range(4)]
                sg = [SB(cs, "sg%d" % i, [128, 256]) for i in range(2)]
                m12 = [SB(cs, "m12_%d" % i, [128, 256]) for i in range(2)]
                mergedT = SB(cs, "mergedT", [128, 8, 256], BF16)
                hbT = SB(cs, "hbT", [128, 8, 256], BF16)
                qT = SB(cs, "qT", [128, 16, 256], BF16)
                sc = SB(cs, "sc", [128, 16, 128])
                wk = SB(cs, "wk", [128, 256])
                stop_ = SB(cs, "stop", [128, 16, 16])
                itopu = SB(cs, "itopu", [128, 16, 16], U32)
                itop = SB(cs, "itop", [128, 16, 16])
                cand = SB(cs, "cand", [128, 8, 256])
                best = SB(cs, "best", [128, 8, 16])
                posu = SB(cs, "posu", [128, 8, 16], U32)
                k1u = SB(cs, "k1u", [128, 128], U32); k2u = SB(cs, "k2u", [128, 128], U32)
                k1f = SB(cs, "k1f", [128, 128]); k2f = SB(cs, "k2f", [128, 128])
                OH = SB(cs, "OH", [128, 128, 16])
                sel = SB(cs, "sel", [128, 3, 128])
                esum = SB(cs, "esum", [128, 8])
                IT = SB(cs, "IT", [128, 3, 256])
                Pm = [SB(cs, "Pm%d" % i, [128, 8, 128], BF16) for i in range(2)]
                Qm = [SB(cs, "Qm%d" % i, [128, 8, 128], BF16) for i in range(2)]
                Gsb = SB(cs, "Gsb", [128, 128, 256], BF16)
                utb = [SB(cs, "utb%d" % i, [128, 8, 128], BF16) for i in range(3)]
                vvb = [SB(cs, "vvb%d" % i, [128, 1024], BF16) for i in range(3)]
                ga = [SB(cs, "ga%d" % i, [128, 256]) for i in range(2)]
                GA = [SB(cs, "GA%d" % i, [128, 256], BF16) for i in range(2)]
                for fc in range(8):
                    P.dma("sp", xres[:, fc % 2, :], din["wo"].ap()[fc], writes=[("xres", fc % 2)])
                    P.op("act", lambda e, fc=fc: e.copy(out=WO[:, fc, :], in_=xres[:, fc % 2, :]), reads=[("xres", fc % 2)], writes=[("WOc", fc)])
                P.dma("sp", AP(cand, 0, [[1, 2048]]), din["skT"].ap(), writes=["cand"])
                P.op("act", lambda e: e.copy(out=SKT[:], in_=AP(cand, 0, [[1, 2048]])), reads=["cand"], writes=["SKT"])
                wctr = {"n": 0}

                def wload(src_t, idx, key):
                    k = wctr["n"]; wctr["n"] += 1
                    P.dma("sp", wst[k % 4][:], src_t.ap()[idx], reads=[key], writes=[("wst", k % 4)])
                    return wst[k % 4], ("wst", k % 4)

                def transposes(src, skey, dstT, dkey, s):
                    for dc in range(8):
                        P.op("pe", lambda e, dc=dc: e.transpose(out=PT[:, dc * 128:(dc + 1) * 128], in_=src[:, dc * 128:(dc + 1) * 128], identity=identb[:]),
                             reads=[skey, "identb"], writes=["PT"])
                    P.op("act", lambda e, s=s: e.copy(out=AP(dstT, s * 128, [[256, 8], [1, 128]]), in_=PT.rearrange("p (a b) -> p a b", a=8)),
                         reads=["PT"], writes=[dkey])

                for tt in range(cfg["NTT"]):
                    T0 = tt * 256
                    for s in range(2):
                        P.dma("sp", xres[:, s, :], DAP(din["x_b"], (T0 + s * 128) * 1024, [[1024, 128], [1, 1024]]), writes=[("xres", s)])
                        rmsnorm_scale(xres[:, s, :], ("xres", s), ss, rs, s, junk, gbc, hbm[:], "hbm")
                        transposes(hbm, "hbm", hT, "hT", s)
                    zkeys = [("zloc", "fn", gi) for gi in range(cfg["NG"])] + [("zloc", "hy", gi, cq) for gi in range(cfg["NG"]) for cq in range(4)]
                    P.dma("sp", zT[:], DAP(zloc, T0, [[2048, 128], [128 * 2048, 8], [1, 256]]), reads=zkeys, writes=["zT"])
                    for fi in range(8):
                        wgh, kgh = wload(WGs, fi, ("WGs", fi))
                        wgf, kgf = wload(WGs, 8 + fi, ("WGs", 8 + fi))
                        wyy, kyy = wload(WYs, fi, ("WYs", fi))
                        pg = PB[0]; py = PB[1]
                        for half, (wt, wk_) in enumerate(((wgh, kgh), (wgf, kgf))):
                            for dc in range(8):
                                P.op("pe", lambda e, half=half, wt=wt, dc=dc: e.matmul(out=pg[:, half * 256:(half + 1) * 256], lhsT=wt[:, dc * 128:(dc + 1) * 128],
                                                                                      rhs=hT[:, dc, :], start=(dc == 0), stop=(dc == 7)),
                                     reads=[wk_, "hT"], writes=[("pb", 0)])
                        for half in range(2):
                            for cc in range(4):
                                P.op("pe", lambda e, half=half, cc=cc, wyy=wyy: e.matmul(out=py[:, half * 256:(half + 1) * 256],
                                                                               lhsT=wyy[:, (half * 4 + cc) * 128:(half * 4 + cc + 1) * 128],
                                                                               rhs=zT[:, half * 4 + cc, :], start=(cc == 0), stop=(cc == 3)),
                                     reads=[kyy, "zT"], writes=[("pb", 1)])
                        for half in range(2):
                            P.op("act", lambda e, half=half, fi=fi: e.activation(out=sg[half][:], in_=pg[:, half * 256:(half + 1) * 256], func=ACT.Sigmoid,
                                                                                bias=bg[:, half * 8 + fi: half * 8 + fi + 1], scale=1.0),
                                 reads=[("pb", 0), "bg"], writes=[("sg", half)])
                            P.op("dve", lambda e, half=half: e.tensor_tensor(out=m12[half][:], in0=sg[half][:], in1=py[:, half * 256:(half + 1) * 256], op=ALU.mult),
                                 reads=[("sg", half), ("pb", 1)], writes=[("m12", half)])
                        P.op("pool", lambda e, fi=fi: e.tensor_add(out=mergedT[:, fi, :], in0=m12[0][:], in1=m12[1][:]),
                             reads=[("m12", 0), ("m12", 1)], writes=["mergedT"])
                    for s in range(2):
                        for half in range(2):
                            po = PB[2 + half]
                            for fc in range(8):
                                P.op("pe", lambda e, s=s, half=half, fc=fc, po=po: e.matmul(out=po[:], lhsT=mergedT[:, fc, s * 128:(s + 1) * 128],
                                                                                          rhs=WO[:, fc, half * 512:(half + 1) * 512], start=(fc == 0), stop=(fc == 7)),
                                     reads=["mergedT", ("WOc", fc)], writes=[("pb", 2 + half)])
                            P.op("dve", lambda e, s=s, half=half, po=po: e.tensor_add(out=xres[:, s, half * 512:(half + 1) * 512],
                                                                                     in0=xres[:, s, half * 512:(half + 1) * 512], in1=po[:]),
                                 reads=[("pb", 2 + half), ("xres", s)], writes=[("xres", s)])
                    if cfg["DEBUG"] and tt == 0:
                        P.op("dve", lambda e: e.tensor_copy(out=AP(cand, 0, [[1, 2048]]), in_=AP(mergedT, 0, [[1, 2048]])), reads=["mergedT"], writes=["cand"])
                        P.dma("sp", dbg["mt"].ap(), AP(cand, 0, [[1, 2048]]), reads=["cand"])
                    if cfg["DEBUG"]:
                        for s in range(2):
                            P.dma("sp", DAP(dbg["xa"], (T0 + s * 128) * 1024, [[1024, 128], [1, 1024]]), xres[:, s, :], reads=[("xres", s)])
                    for s in range(2):
                        rmsnorm_scale(xres[:, s, :], ("xres", s), ss, rs, s, junk, gbf, hbm[:], "hbm")
                        transposes(hbm, "hbm", hbT, "hbT", s)
                    for hc in range(16):
                        wq_, kq_ = wload(WQs, hc, ("WQs", hc))
                        pq = PB[hc % 2]
                        for dc in range(8):
                            P.op("pe", lambda e, wq_=wq_, dc=dc, pq=pq: e.matmul(out=pq[:, 0:256], lhsT=wq_[:, dc * 128:(dc + 1) * 128], rhs=hbT[:, dc, :],
                                                                                start=(dc == 0), stop=(dc == 7)),
                                 reads=[kq_, "hbT"], writes=[("pb", hc % 2)])
                        P.op("act", lambda e, hc=hc, pq=pq: e.copy(out=qT[:, hc, :], in_=pq[:, 0:256]), reads=[("pb", hc % 2)], writes=["qT"])
                    for s in range(2):
                        for hc in range(16):
                            P.op("pe", lambda e, s=s, hc=hc: e.matmul(out=PB[2 + hc // 4][:, (hc % 4) * 128:(hc % 4 + 1) * 128], lhsT=qT[:, hc, s * 128:(s + 1) * 128],
                                                                     rhs=SKT[:, hc * 128:(hc + 1) * 128], start=True, stop=True),
                                 reads=["qT", "SKT"], writes=[("pb", 2 + hc // 4)])
                        for b4 in range(4):
                            P.op("act", lambda e, b4=b4: e.copy(out=AP(sc, b4 * 512, [[1, 512]]), in_=PB[2 + b4][:]), reads=[("pb", 2 + b4)], writes=["sc"])
                        SK_ = [("stop", hc) for hc in range(16)]
                        IK_ = [("itopu", hc) for hc in range(16)]
                        for hp in range(8):
                            hcs = (2 * hp, 2 * hp + 1)
                            for hc in hcs:
                                P.op("dve", lambda e, hc=hc: e.max(out=stop_[:, hc, 0:8], in_=sc[:, hc, :]), reads=["sc"], writes=[("stop", hc)])
                            for hc in hcs:
                                P.op("dve", lambda e, hc=hc: e.max_index(out=itopu[:, hc, 0:8], in_max=stop_[:, hc, 0:8], in_values=sc[:, hc, :]),
                                     reads=["sc", ("stop", hc)], writes=[("itopu", hc)])
                            for w, hc in enumerate(hcs):
                                P.op("dve", lambda e, hc=hc, w=w: e.match_replace(out=wk[:, w * 128:(w + 1) * 128], in_to_replace=stop_[:, hc, 0:8],
                                                                                   in_values=sc[:, hc, :], imm_value=-1e30),
                                     reads=["sc", ("stop", hc)], writes=[("wk", w)])
                            for w, hc in enumerate(hcs):
                                P.op("dve", lambda e, hc=hc, w=w: e.max(out=stop_[:, hc, 8:16], in_=wk[:, w * 128:(w + 1) * 128]),
                                     reads=[("wk", w)], writes=[("stop", hc)])
                            for w, hc in enumerate(hcs):
                                P.op("dve", lambda e, hc=hc, w=w: e.max_index(out=itopu[:, hc, 8:16], in_max=stop_[:, hc, 8:16], in_values=wk[:, w * 128:(w + 1) * 128]),
                                     reads=[("wk", w), ("stop", hc)], writes=[("itopu", hc)])
                        P.op("dve", lambda e: e.tensor_copy(out=itop[:], in_=itopu[:]), reads=IK_, writes=["itop"])
                        P.op("pool", lambda e: e.tensor_tensor(out=AP(cand, 0, [[256, 8], [16, 16], [1, 16]]),
                                                              in0=AP(stop_, 0, [[32, 8], [1, 16], [0, 16]]),
                                                              in1=AP(stop_, 16, [[32, 8], [0, 16], [1, 16]]), op=ALU.add),
                             reads=SK_, writes=["cand"])
                        wkb = [wk[:], AP(OH, 0, [[1, 256]])]
                        wkk = [[("wk", 0), ("wk", 1)], ["OH"]]
                        for hp in range(4):
                            hs = (2 * hp, 2 * hp + 1)
                            for h in hs:
                                P.op("dve", lambda e, h=h: e.max(out=best[:, h, 0:8], in_=cand[:, h, :]), reads=["cand"], writes=[("best", h)])
                            for h in hs:
                                P.op("dve", lambda e, h=h: e.max_index(out=posu[:, h, 0:8], in_max=best[:, h, 0:8], in_values=cand[:, h, :]),
                                     reads=["cand", ("best", h)], writes=[("posu", h)])
                            for w, h in enumerate(hs):
                                P.op("dve", lambda e, h=h, w=w: e.match_replace(out=wkb[w], in_to_replace=best[:, h, 0:8], in_values=cand[:, h, :], imm_value=-1e30),
                                     reads=["cand", ("best", h)], writes=wkk[w])
                            for w, h in enumerate(hs):
                                P.op("dve", lambda e, h=h, w=w: e.max(out=best[:, h, 8:16], in_=wkb[w]), reads=wkk[w], writes=[("best", h)])
                            for w, h in enumerate(hs):
                                P.op("dve", lambda e, h=h, w=w: e.max_index(out=posu[:, h, 8:16], in_max=best[:, h, 8:16], in_values=wkb[w]),
                                     reads=wkk[w] + [("best", h)], writes=[("posu", h)])
                        BK_ = [("best", h) for h in range(8)]
                        PK_ = [("posu", h) for h in range(8)]
                        P.op("dve", lambda e: e.tensor_single_scalar(out=k1u[:], in_=AP(posu, 0, [[1, 128]]), scalar=4, op=ALU.logical_shift_right),
                             reads=PK_, writes=["k1u"])
                        P.op("dve", lambda e: e.tensor_single_scalar(out=k2u[:], in_=AP(posu, 0, [[1, 128]]), scalar=15, op=ALU.bitwise_and),
                             reads=PK_, writes=["k2u"])
                        P.op("dve", lambda e: e.tensor_copy(out=k1f[:], in_=k1u[:]), reads=["k1u"], writes=["k1f"])
                        P.op("dve", lambda e: e.tensor_copy(out=k2f[:], in_=k2u[:]), reads=["k2u"], writes=["k2f"])
                        for which, kf_ in enumerate((k1f, k2f)):
                            P.op("dve", lambda e, kf_=kf_: e.tensor_tensor(out=OH[:], in0=AP(kf_, 0, [[1, 128], [0, 16]]),
                                                                            in1=AP(iota16, 0, [[0, 128], [1, 16]]), op=ALU.is_equal),
                                 reads=["k1f", "k2f", "iota16"], writes=["OH"])
                            P.op("pool", lambda e, which=which: e.tensor_tensor(out=AP(OH, 0, [[256, 8], [16, 16], [1, 16]]),
                                                                               in0=AP(OH, 0, [[256, 8], [16, 16], [1, 16]]),
                                                                               in1=AP(itop, which * 16, [[32, 8], [0, 16], [1, 16]]), op=ALU.mult),
                                 reads=["OH", "itop"], writes=["OH"])
                            P.op("dve", lambda e, which=which: e.tensor_reduce(out=sel[:, which, :], in_=OH[:], axis=AX.X, op=ALU.add),
                                 reads=["OH"], writes=[("sel", which)])
                        P.op("dve", lambda e: e.tensor_tensor(out=AP(sel, 256, [[16, 8], [1, 16]]), in0=best[:],
                                                              in1=AP(best, 0, [[16, 8], [0, 16]]), op=ALU.subtract),
                             reads=BK_, writes=[("sel", 2)])
                        P.op("act", lambda e: e.activation(out=sel[:, 2, :], in_=sel[:, 2, :], func=ACT.Exp), reads=[("sel", 2)], writes=[("sel", 2)])
                        P.op("dve", lambda e: e.tensor_reduce(out=esum[:], in_=AP(sel, 256, [[16, 8], [1, 16]]), axis=AX.X, op=ALU.add),
                             reads=[("sel", 2)], writes=["esum"])
                        P.op("dve", lambda e: e.reciprocal(out=esum[:], in_=esum[:]), reads=["esum"], writes=["esum"])
                        P.op("dve", lambda e: e.tensor_tensor(out=AP(sel, 256, [[16, 8], [1, 16]]), in0=AP(sel, 256, [[16, 8], [1, 16]]),
                                                              in1=AP(esum, 0, [[1, 8], [0, 16]]), op=ALU.mult),
                             reads=[("sel", 2), "esum"], writes=[("sel", 2)])
                        for which in range(3):
                            P.op("pe", lambda e, which=which: e.transpose(out=PB[6][:, which * 128:(which + 1) * 128], in_=sel[:, which, :], identity=identf[:]),
                                 reads=[("sel", which), "identf"], writes=[("pb", 6)])
                        P.op("act", lambda e, s=s: e.copy(out=AP(IT, s * 128, [[256, 3], [1, 128]]), in_=AP(PB[6], 0, [[128, 3], [1, 128]])),
                             reads=[("pb", 6)], writes=["IT"])
                    TB = 8
                    for blk in range(256 // TB):
                        t0 = blk * TB
                        bb = blk % 2
                        P.op("dve", lambda e, t0=t0, bb=bb: e.tensor_tensor(out=Pm[bb][:], in0=AP(iota128, 0, [[0, TB], [1, 128]]),
                                                                         in1=AP(IT, t0, [[1, TB], [0, 128]]), op=ALU.is_equal),
                             reads=["iota128", "IT"], writes=[("Pm", bb)])
                        P.op("dve", lambda e, t0=t0, bb=bb: e.tensor_tensor(out=Qm[bb][:], in0=AP(iota128, 0, [[0, TB], [1, 128]]),
                                                                         in1=AP(IT, 256 + t0, [[1, TB], [0, 128]]), op=ALU.is_equal),
                             reads=["iota128", "IT"], writes=[("Qm", bb)])
                        P.op("pool", lambda e, t0=t0, bb=bb: e.tensor_tensor(out=Qm[bb][:], in0=Qm[bb][:],
                                                                          in1=AP(IT, 512 + t0, [[1, TB], [0, 128]]), op=ALU.mult),
                             reads=[("Qm", bb), "IT"], writes=[("Qm", bb)])
                        for tl in range(TB):
                            t = t0 + tl
                            r4 = t % 4
                            gq = (t // 4) % 2
                            pGb = PB[gq]
                            P.op("pe", lambda e, r4=r4, pGb=pGb, tl=tl, bb=bb: e.matmul(out=pGb[:, r4 * 128:(r4 + 1) * 128], lhsT=Qm[bb][:, tl, :], rhs=Pm[bb][:, tl, :],
                                                                                   start=True, stop=True),
                                 reads=[("Pm", bb), ("Qm", bb)], writes=[("pb", gq)])
                            if r4 == 3:
                                tq = t - 3
                                if (t // 4) % 4 != 3:
                                    P.op("act", lambda e, tq=tq, pGb=pGb: e.copy(out=AP(Gsb, tq, [[1, 4], [256, 128]]), in_=AP(pGb, 0, [[128, 4], [1, 128]])),
                                         reads=[("pb", gq)], writes=["Gsb_a"])
                                else:
                                    P.op("dve", lambda e, tq=tq, pGb=pGb: e.tensor_copy(out=AP(Gsb, tq, [[1, 4], [256, 128]]), in_=AP(pGb, 0, [[128, 4], [1, 128]])),
                                         reads=[("pb", gq)], writes=["Gsb_d"])
                    P.flush()
                    nch = cfg["NCH"]

                    def loadw(i):
                        P.dma("sp", AP(utb[i % 3], 0, [[1, 1024]]), UTs.ap()[i], reads=[("UTs", i)], writes=[("utb", i % 3)])
                        P.dma("sp", vvb[i % 3][:], Vs.ap()[i], reads=[("Vs", i)], writes=[("vvb", i % 3)])

                    def amm(i):
                        pa_ = PB[i % 2]
                        for dc in range(8):
                            P.op("pe", lambda e, i=i, dc=dc, pa_=pa_: e.matmul(out=pa_[:, 0:256], lhsT=utb[i % 3][:, dc, :], rhs=hbT[:, dc, :],
                                                                              start=(dc == 0), stop=(dc == 7)),
                                 reads=[("utb", i % 3), "hbT"], writes=[("pb", i % 2)])

                    loadw(0)
                    if nch > 1:
                        loadw(1)
                    amm(0)
                    for i in range(nch):
                        if i + 2 < nch:
                            loadw(i + 2)
                        if i + 1 < nch:
                            amm(i + 1)
                        P.op("act", lambda e, i=i: e.activation(out=ga[i % 2][:], in_=PB[i % 2][:, 0:256], func=ACT.Gelu),
                             reads=[("pb", i % 2)], writes=[("ga", i % 2)])
                        eng = "dve" if i % 2 == 0 else "pool"
                        P.op(eng, lambda e, i=i: e.tensor_tensor(out=GA[i % 2][:], in0=ga[i % 2][:], in1=Gsb[:, i, :], op=ALU.mult),
                             reads=[("ga", i % 2), "Gsb_a", "Gsb_d"], writes=[("GA", i % 2)])
                        for s in range(2):
                            for half in range(2):
                                P.op("pe", lambda e, i=i, s=s, half=half: e.matmul(out=PB[2 + s * 2 + half][:], lhsT=GA[i % 2][:, s * 128:(s + 1) * 128],
                                                                                  rhs=vvb[i % 3][:, half * 512:(half + 1) * 512],
                                                                                  start=(i == 0), stop=(i == nch - 1)),
                                     reads=[("GA", i % 2), ("vvb", i % 3)], writes=[("pb", 2 + s * 2 + half)])
                    for s in range(2):
                        for half in range(2):
                            P.op("dve", lambda e, s=s, half=half: e.tensor_add(out=xres[:, s, half * 512:(half + 1) * 512],
                                                                              in0=xres[:, s, half * 512:(half + 1) * 512], in1=PB[2 + s * 2 + half][:]),
                                 reads=[("pb", 2 + s * 2 + half), ("xres", s)], writes=[("xres", s)])
                        P.op("act", lambda e, s=s: e.activation(out=junk[:], in_=xres[:, s, :], func=ACT.Square, accum_out=ss[:, s:s + 1]),
                             reads=[("xres", s)], writes=["junk", ("ss", s)])
                        P.op("act", lambda e, s=s: e.activation(out=rs[:, s:s + 1], in_=ss[:, s:s + 1], func=ACT.Sqrt, scale=1.0 / 1024.0, bias=epsT[:]),
                             reads=[("ss", s), "epsT"], writes=[("rs", s)])
                        P.op("dve", lambda e, s=s: e.reciprocal(out=rs[:, s:s + 1], in_=rs[:, s:s + 1]), reads=[("rs", s)], writes=[("rs", s)])
                        P.op("dve", lambda e, s=s: e.scalar_tensor_tensor(out=AP(sc, s * 1024, [[1, 1024]]), in0=xres[:, s, :], scalar=rs[:, s:s + 1], in1=gfin[:],
                                                                         op0=ALU.mult, op1=ALU.mult),
                             reads=[("xres", s), ("rs", s), "gfin"], writes=["sc"])
                        P.dma("sp", DAP(out_t, (T0 + s * 128) * 1024, [[1024, 128], [1, 1024]]), AP(sc, s * 1024, [[1, 1024]]), reads=["sc"])
                    P.flush()
        P.wait_all_dma("sp")
        P.flush()
    return nc


def make_inputs(x, norm_mix_g, w_in, b_in, conv_w, conv_b, filt_w1, filt_b1, filt_w2, filt_b2, filt_w3,
                hyena_skip, w_hyena_out, w_fnet_out, w_out, norm_ffn_g, peer_w_q, peer_sub_keys,
                peer_u, peer_v, norm_final_g):
    f = lambda a: np.ascontiguousarray(np.asarray(a, dtype=np.float32))
    x = f(x); w_in = f(w_in)[0]; b_in = f(b_in)[0]; conv_w = f(conv_w)[0]; conv_b = f(conv_b)[0]
    shared = {}
    shared["gmix"] = f(norm_mix_g)[0][None, :]
    shared["gffn"] = f(norm_ffn_g)[0][None, :]
    shared["gfin"] = f(norm_final_g)[None, :]
    wa = np.zeros((4, 128, 8, 384), np.float32)
    cw = np.zeros((4, 3, 384), np.float32); cb = np.zeros((4, 1, 384), np.float32); binh = np.zeros((4, 1, 384), np.float32)
    wfT = np.zeros((4, 128, 1024), np.float32); bfn = np.zeros((4, 128, 1), np.float32)
    w3 = np.zeros((4, 64, 512), np.float32); skip = np.zeros((4, 1, 256), np.float32)
    fw3 = f(filt_w3)[0]; hs = f(hyena_skip)[0]
    for g in range(4):
        cols = np.concatenate([np.arange(128) + g * 128 + kind * 512 for kind in range(3)])
        wa[g] = w_in[:, cols].reshape(8, 128, 384).transpose(1, 0, 2)
        cw[g] = conv_w[:, cols]; cb[g, 0] = conv_b[cols]; binh[g, 0] = b_in[cols]
        fc = 1536 + g * 128 + np.arange(128)
        wfT[g] = w_in[:, fc].T; bfn[g, :, 0] = b_in[fc]
        w3c = np.concatenate([o * 1024 + dr * 512 + g * 128 + np.arange(128) for o in range(2) for dr in range(2)])
        w3[g] = fw3[:, w3c]
        skip[g, 0] = hs[:, g * 128:(g + 1) * 128].reshape(-1)
    shared.update(wa=wa, cw=cw, cb=cb, binh=binh, wfT=wfT, bfn=bfn, w3=w3, skip=skip)
    shared["fw1"] = f(filt_w1)[0]; shared["fb1"] = f(filt_b1)[0][:, None]
    shared["fw2"] = f(filt_w2)[0]; shared["fb2"] = f(filt_b2)[0][:, None]
    wgc = w_in[:, 2048:4096]
    shared["wg"] = np.ascontiguousarray(wgc.reshape(8, 128, 16, 128).transpose(2, 1, 0, 3).reshape(16, 128, 1024))
    shared["bg"] = np.ascontiguousarray(b_in[2048:4096].reshape(16, 128).T)
    why = f(w_hyena_out)[0]; wfn = f(w_fnet_out)[0]
    wy = np.zeros((8, 128, 2, 4, 128), np.float32)
    wy[:, :, 0] = why.reshape(4, 128, 8, 128).transpose(2, 1, 0, 3)
    wy[:, :, 1] = wfn.reshape(4, 128, 8, 128).transpose(2, 1, 0, 3)
    shared["wy"] = wy.reshape(8, 128, 1024)
    shared["wo"] = f(w_out)[0].reshape(8, 128, 1024)
    wq = f(peer_w_q)[0]
    shared["wq"] = np.ascontiguousarray(wq.reshape(8, 128, 16, 128).transpose(2, 1, 0, 3).reshape(16, 128, 1024))
    sk = f(peer_sub_keys)[0]
    shared["skT"] = np.ascontiguousarray(sk.reshape(16, 128, 128).transpose(2, 0, 1).reshape(128, 2048))
    pu = f(peer_u)[0]
    shared["ut"] = np.ascontiguousarray(pu.reshape(128, 128, 8, 128).transpose(0, 3, 2, 1).reshape(128, 128, 1024))
    shared["pv"] = f(peer_v)[0].reshape(128, 128, 1024)
    in_maps = []
    for r in range(NCORES):
        b, q = r // 4, r % 4
        m = dict(shared)
        m["x_b"] = np.ascontiguousarray(np.roll(x[b], -2048 * q, axis=0))
        m.update(host_consts(q))
        in_maps.append(m)
    return in_maps


def kernel(**inputs):
    in_maps = make_inputs(**inputs)
    nc = build()
    res = run_bass_kernel_spmd(nc, in_maps, core_ids=list(range(NCORES)))
    out = np.zeros((2, L, D), np.float32)
    for r in range(NCORES):
        b, q = r // 4, r % 4
        out[b, 2048 * q:2048 * (q + 1)] = res.results[r]["out"]
    return out
```
